# Optimizing a Trainium2 kernel written in Bass

```python
import math
import jax, jax.numpy as jnp
from jax import lax
import numpy as np

D_MODEL = 1024
BATCH = 4
SEQ = 8192
DEPTH = 4

N_A = DEPTH // 2
N_B = DEPTH - N_A
EPS = 1e-6

D_FF = ((8 * D_MODEL + 3 * 256 - 1) // (3 * 256)) * 256

SSM_EXPAND = 2
D_INNER = SSM_EXPAND * D_MODEL
SSM_HEADDIM = 64
SSM_HEADS = D_INNER // SSM_HEADDIM
SSM_STATE = 128
SSM_GROUPS = 4
SSM_HPG = SSM_HEADS // SSM_GROUPS
CONV_W = 4
SSD_CHUNK = 256
D_XBC = D_INNER + 2 * SSM_GROUPS * SSM_STATE
D_SSM_IN = D_INNER + D_XBC + SSM_HEADS

NSA_HEADS = 16
NSA_KV_HEADS = 4
NSA_GROUP = NSA_HEADS // NSA_KV_HEADS
NSA_HEAD_DIM = 64
CMP_BLOCK = 32
CMP_STRIDE = 16
CMP_HIDDEN = 256
SEL_BLOCK = 64
N_SEL = 16
WINDOW = 512
Q_BLOCK = 128
N_BRANCH = 3
D_NSA_Q = NSA_HEADS * NSA_HEAD_DIM + NSA_HEADS * N_BRANCH
D_KV = 6 * NSA_KV_HEADS * NSA_HEAD_DIM

ROPE_THETA = 500000.0
ROT_DIMS = NSA_HEAD_DIM // 4

kernel_name = "yoco_mamba2_nsa_hybrid"


def rmsnorm(x, g):
    xf = x.astype(jnp.float32)
    y = xf * lax.rsqrt(jnp.mean(xf * xf, axis=-1, keepdims=True) + EPS)
    return (y * g.astype(jnp.float32)).astype(x.dtype)


def adaln(x, g, shift, scale):
    return rmsnorm(x, g) * (1 + scale[:, None, :]) + shift[:, None, :]


def partial_rope(x, pos):
    half = ROT_DIMS // 2
    inv_freq = ROPE_THETA ** (-jnp.arange(half, dtype=jnp.float32) / half)
    ang = pos.astype(jnp.float32)[:, None] * inv_freq[None, :]
    bshape = (1, pos.shape[0]) + (1,) * (x.ndim - 3) + (half,)
    cos = jnp.cos(ang).reshape(bshape)
    sin = jnp.sin(ang).reshape(bshape)
    x1 = x[..., :half].astype(jnp.float32)
    x2 = x[..., half:ROT_DIMS].astype(jnp.float32)
    rot = jnp.concatenate([x1 * cos - x2 * sin, x2 * cos + x1 * sin], axis=-1).astype(x.dtype)
    return jnp.concatenate([rot, x[..., ROT_DIMS:]], axis=-1)


def masked_softmax(s, mask, axis):
    s = jnp.where(mask, s.astype(jnp.float32), -jnp.inf)
    m = jnp.max(s, axis=axis, keepdims=True)
    m = jnp.where(jnp.isfinite(m), m, 0.0)
    p = jnp.exp(s - m)
    d = jnp.sum(p, axis=axis, keepdims=True)
    return p / jnp.where(d > 0, d, 1.0)


def swiglu(h, w_in, w_out):
    a, b = jnp.split(h @ w_in, 2, axis=-1)
    return (jax.nn.silu(a) * b) @ w_out


def causal_dwconv(x, w, b):
    y = lax.conv_general_dilated(
        x, w[:, None, :].astype(x.dtype), window_strides=(1,),
        padding=[(CONV_W - 1, 0)], dimension_numbers=("NWC", "WIO", "NWC"),
        feature_group_count=x.shape[-1])
    return y + b


def ssd_scan(x, bm, cm, dt_raw, dt_bias, a_log):
    f32 = jnp.float32
    bsz, s_len, n_h, p_dim = x.shape
    L = math.gcd(s_len, SSD_CHUNK)
    nc = s_len // L
    G, J, N = SSM_GROUPS, SSM_HPG, SSM_STATE
    A = -jnp.exp(a_log.astype(f32))
    dt = jax.nn.softplus(dt_raw.astype(f32) + dt_bias.astype(f32))

    def to_chunks(a):
        return jnp.moveaxis(a.astype(f32).reshape((bsz, nc, L) + a.shape[2:]), 1, 0)

    causal = jnp.tril(jnp.ones((L, L), dtype=bool))

    def step(state, inp):
        xk, bk, ck, dtk = inp
        acs = jnp.cumsum(dtk * A, axis=1)
        seg = acs[:, :, None, :] - acs[:, None, :, :]
        decay = jnp.exp(jnp.where(causal[None, :, :, None], seg, -jnp.inf)).reshape(bsz, L, L, G, J)
        xdt = (xk * dtk[..., None]).reshape(bsz, L, G, J, p_dim)
        cb = jnp.einsum("blgn,bsgn->blsg", ck, bk)
        y_intra = jnp.einsum("blsg,blsgj,bsgjp->blgjp", cb, decay, xdt)
        y_inter = jnp.einsum("blgn,bgjpn->blgjp", ck, state) * jnp.exp(acs).reshape(bsz, L, G, J)[..., None]
        tail = jnp.exp(acs[:, -1:, :] - acs).reshape(bsz, L, G, J)
        state = (state * jnp.exp(acs[:, -1]).reshape(bsz, G, J)[..., None, None]
                 + jnp.einsum("bsgn,bsgj,bsgjp->bgjpn", bk, tail, xdt))
        return state, (y_intra + y_inter).reshape(bsz, L, n_h, p_dim)

    state0 = jnp.zeros((bsz, G, J, p_dim, N), f32)
    _, ys = lax.scan(step, state0, (to_chunks(x), to_chunks(bm), to_chunks(cm), to_chunks(dt)))
    return jnp.moveaxis(ys, 0, 1).reshape(bsz, s_len, n_h, p_dim)


def mamba2_mixer(h, w_in, conv_w, conv_b, dt_bias, a_log, d_skip, norm_g, w_out):
    bsz, s_len, _ = h.shape
    proj = h @ w_in
    z, xbc, dt = jnp.split(proj, [D_INNER, D_INNER + D_XBC], axis=-1)
    xbc = jax.nn.silu(causal_dwconv(xbc, conv_w, conv_b))
    xs, bm, cm = jnp.split(xbc, [D_INNER, D_INNER + SSM_GROUPS * SSM_STATE], axis=-1)
    xs = xs.reshape(bsz, s_len, SSM_HEADS, SSM_HEADDIM)
    y = ssd_scan(xs, bm.reshape(bsz, s_len, SSM_GROUPS, SSM_STATE),
                 cm.reshape(bsz, s_len, SSM_GROUPS, SSM_STATE), dt, dt_bias, a_log)
    y = (y + d_skip.astype(jnp.float32)[:, None] * xs.astype(jnp.float32)).astype(h.dtype)
    y = y.reshape(bsz, s_len, D_INNER) * jax.nn.silu(z)
    y = rmsnorm(y.reshape(bsz, s_len, SSM_GROUPS, D_INNER // SSM_GROUPS),
                norm_g.reshape(SSM_GROUPS, D_INNER // SSM_GROUPS))
    return y.reshape(bsz, s_len, D_INNER) @ w_out


def nsa_shared_kv(stream, cs, kv_norm, kv_mod_w, kv_mod_b, w_kv,
                  cmp_pos_k, cmp_w1_k, cmp_w2_k, cmp_pos_v, cmp_w1_v, cmp_w2_v):
    bsz, s_len, _ = stream.shape
    shift, scale = jnp.split(cs @ kv_mod_w + kv_mod_b, 2, axis=-1)
    h = adaln(stream, kv_norm, shift, scale)
    kv = (h @ w_kv).reshape(bsz, s_len, 6, NSA_KV_HEADS, NSA_HEAD_DIM)
    k_c, v_c, k_s, v_s, k_w, v_w = [kv[:, :, i] for i in range(6)]
    pos = jnp.arange(s_len)
    n_cmp = (s_len - CMP_BLOCK) // CMP_STRIDE + 1
    idx = jnp.arange(n_cmp)[:, None] * CMP_STRIDE + jnp.arange(CMP_BLOCK)[None, :]

    def compress(t, pe, w1, w2):
        blk = t[:, idx] + pe[None, None, :, None, :]
        blk = jnp.moveaxis(blk, 3, 2).reshape(bsz, n_cmp, NSA_KV_HEADS, CMP_BLOCK * NSA_HEAD_DIM)
        return jax.nn.silu(blk @ w1) @ w2

    k_cmp = compress(k_c, cmp_pos_k, cmp_w1_k, cmp_w2_k)
    v_cmp = compress(v_c, cmp_pos_v, cmp_w1_v, cmp_w2_v)
    nsb = s_len // SEL_BLOCK
    k_sel = partial_rope(k_s, pos).reshape(bsz, nsb, SEL_BLOCK, NSA_KV_HEADS, NSA_HEAD_DIM).transpose(0, 3, 1, 2, 4)
    v_sel = v_s.reshape(bsz, nsb, SEL_BLOCK, NSA_KV_HEADS, NSA_HEAD_DIM).transpose(0, 3, 1, 2, 4)
    pad = ((0, 0), (WINDOW, 0), (0, 0), (0, 0))
    k_win = jnp.pad(partial_rope(k_w, pos), pad)
    v_win = jnp.pad(v_w, pad)
    return k_cmp, v_cmp, k_sel, v_sel, k_win, v_win


def nsa_mixer(h, w_q, w_o, k_cmp, v_cmp, k_sel, v_sel, k_win, v_win):
    bsz, s_len, _ = h.shape
    nq = s_len // Q_BLOCK
    nsb = s_len // SEL_BLOCK
    n_sel = min(N_SEL, nsb)
    n_cmp = k_cmp.shape[1]
    n_units = s_len // CMP_STRIDE
    units = CMP_BLOCK // CMP_STRIDE
    scale = NSA_HEAD_DIM ** -0.5
    proj = h @ w_q
    q = proj[..., :NSA_HEADS * NSA_HEAD_DIM].reshape(bsz, s_len, NSA_KV_HEADS, NSA_GROUP, NSA_HEAD_DIM)
    gates = jax.nn.sigmoid(proj[..., NSA_HEADS * NSA_HEAD_DIM:].astype(jnp.float32)).reshape(
        bsz, s_len, NSA_KV_HEADS, NSA_GROUP, N_BRANCH)
    q_rot = partial_rope(q, jnp.arange(s_len))
    cmp_end = jnp.arange(n_cmp) * CMP_STRIDE + CMP_BLOCK - 1

    def blocks(a):
        return jnp.moveaxis(a.reshape((bsz, nq, Q_BLOCK) + a.shape[2:]), 1, 0)

    def one_block(args):
        qi, qc, qr, g = args
        t = qi * Q_BLOCK + jnp.arange(Q_BLOCK)
        s = jnp.einsum("bqhgd,bchd->bhgqc", qc, k_cmp) * scale
        p_cmp = masked_softmax(s, (cmp_end[None, :] <= t[:, None])[None, None, None], axis=-1)
        o_cmp = jnp.einsum("bhgqc,bchd->bqhgd", p_cmp.astype(v_cmp.dtype), v_cmp)
        p_grp = p_cmp.sum(axis=2)
        u = sum(jnp.pad(p_grp, ((0, 0), (0, 0), (0, 0), (r, n_units - n_cmp - r))) for r in range(units))
        imp = u.reshape(bsz, NSA_KV_HEADS, Q_BLOCK, nsb, SEL_BLOCK // CMP_STRIDE).sum(axis=-1)
        qblk = t // SEL_BLOCK
        j = jnp.arange(nsb)
        forced = (j[None] == 0) | (j[None] == qblk[:, None]) | (j[None] == qblk[:, None] - 1)
        causal_blk = j[None] <= qblk[:, None]
        score = jnp.where(forced, jnp.inf, jnp.where(causal_blk, imp, -1.0))
        _, sel = lax.top_k(score, n_sel)
        gather = jax.vmap(jax.vmap(lambda kk, ii: kk[ii]))
        ks = gather(k_sel, sel)
        vs = gather(v_sel, sel)
        tok = sel[..., None] * SEL_BLOCK + jnp.arange(SEL_BLOCK)
        s = jnp.einsum("bqhgd,bhqnkd->bhgqnk", qr, ks) * scale
        p = masked_softmax(s, (tok <= t[None, None, :, None, None])[:, :, None], axis=(-2, -1))
        o_sel = jnp.einsum("bhgqnk,bhqnkd->bqhgd", p.astype(vs.dtype), vs)
        kw = lax.dynamic_slice_in_dim(k_win, qi * Q_BLOCK, WINDOW + Q_BLOCK, axis=1)
        vw = lax.dynamic_slice_in_dim(v_win, qi * Q_BLOCK, WINDOW + Q_BLOCK, axis=1)
        kp = qi * Q_BLOCK - WINDOW + jnp.arange(WINDOW + Q_BLOCK)
        wmask = (kp[None] <= t[:, None]) & (kp[None] > t[:, None] - WINDOW) & (kp[None] >= 0)
        s = jnp.einsum("bqhgd,bkhd->bhgqk", qr, kw) * scale
        p = masked_softmax(s, wmask[None, None, None], axis=-1)
        o_win = jnp.einsum("bhgqk,bkhd->bqhgd", p.astype(vw.dtype), vw)
        o = g[..., 0:1] * o_cmp + g[..., 1:2] * o_sel + g[..., 2:3] * o_win
        return o.astype(h.dtype)

    o = lax.map(one_block, (jnp.arange(nq), blocks(q), blocks(q_rot), blocks(gates)))
    o = jnp.moveaxis(o, 0, 1).reshape(bsz, s_len, NSA_HEADS * NSA_HEAD_DIM)
    return o @ w_o


def setup_inputs(seed: int = 0) -> dict:
    key = jax.random.key(seed)
    ks = iter(jax.random.split(key, 48))
    f32 = jnp.float32
    D = D_MODEL

    def nrm(shape, scale):
        return jax.random.normal(next(ks), shape, f32) * scale

    def gain(shape):
        return 1.0 + nrm(shape, 0.05)

    x = nrm((BATCH, SEQ, D), 1.0)
    c = nrm((BATCH, D), 1.0)
    gate_offset = jnp.tile(jnp.repeat(jnp.array([0.0, 0.0, 1.0], f32), D), 2)
    mod_w = nrm((DEPTH, D, 6 * D), 0.1 * D ** -0.5)
    mod_b = nrm((DEPTH, 6 * D), 0.02) + gate_offset
    norm_pre_mix = gain((DEPTH, D))
    norm_post_mix = gain((DEPTH, D))
    norm_pre_ffn = gain((DEPTH, D))
    norm_post_ffn = gain((DEPTH, D))
    ffn_w_in = nrm((DEPTH, D, 2 * D_FF), D ** -0.5)
    ffn_w_out = nrm((DEPTH, D_FF, D), D_FF ** -0.5)
    ssm_w_in = nrm((N_A, D, D_SSM_IN), D ** -0.5)
    ssm_conv_w = nrm((N_A, CONV_W, D_XBC), CONV_W ** -0.5)
    ssm_conv_b = nrm((N_A, D_XBC), 0.02)
    dt0 = jnp.exp(jax.random.uniform(next(ks), (N_A, SSM_HEADS), f32, math.log(1e-3), math.log(1e-1)))
    ssm_dt_bias = dt0 + jnp.log(-jnp.expm1(-dt0))
    ssm_a_log = jnp.log(jax.random.uniform(next(ks), (N_A, SSM_HEADS), f32, 1.0, 16.0))
    ssm_d = gain((N_A, SSM_HEADS))
    ssm_norm = gain((N_A, D_INNER))
    ssm_w_out = nrm((N_A, D_INNER, D), D_INNER ** -0.5)
    kv_norm = gain((D,))
    kv_mod_w = nrm((D, 2 * D), 0.1 * D ** -0.5)
    kv_mod_b = nrm((2 * D,), 0.02)
    w_kv = nrm((D, D_KV), D ** -0.5)
    cmp_pos_k = nrm((CMP_BLOCK, NSA_HEAD_DIM), 0.1)
    cmp_w1_k = nrm((CMP_BLOCK * NSA_HEAD_DIM, CMP_HIDDEN), (CMP_BLOCK * NSA_HEAD_DIM) ** -0.5)
    cmp_w2_k = nrm((CMP_HIDDEN, NSA_HEAD_DIM), CMP_HIDDEN ** -0.5)
    cmp_pos_v = nrm((CMP_BLOCK, NSA_HEAD_DIM), 0.1)
    cmp_w1_v = nrm((CMP_BLOCK * NSA_HEAD_DIM, CMP_HIDDEN), (CMP_BLOCK * NSA_HEAD_DIM) ** -0.5)
    cmp_w2_v = nrm((CMP_HIDDEN, NSA_HEAD_DIM), CMP_HIDDEN ** -0.5)
    nsa_w_q = nrm((N_B, D, D_NSA_Q), D ** -0.5)
    nsa_w_o = nrm((N_B, NSA_HEADS * NSA_HEAD_DIM, D), (NSA_HEADS * NSA_HEAD_DIM) ** -0.5)
    return {"x": x, "c": c, "mod_w": mod_w, "mod_b": mod_b,
            "norm_pre_mix": norm_pre_mix, "norm_post_mix": norm_post_mix,
            "norm_pre_ffn": norm_pre_ffn, "norm_post_ffn": norm_post_ffn,
            "ffn_w_in": ffn_w_in, "ffn_w_out": ffn_w_out,
            "ssm_w_in": ssm_w_in, "ssm_conv_w": ssm_conv_w, "ssm_conv_b": ssm_conv_b,
            "ssm_dt_bias": ssm_dt_bias, "ssm_a_log": ssm_a_log, "ssm_d": ssm_d,
            "ssm_norm": ssm_norm, "ssm_w_out": ssm_w_out,
            "kv_norm": kv_norm, "kv_mod_w": kv_mod_w, "kv_mod_b": kv_mod_b, "w_kv": w_kv,
            "cmp_pos_k": cmp_pos_k, "cmp_w1_k": cmp_w1_k, "cmp_w2_k": cmp_w2_k,
            "cmp_pos_v": cmp_pos_v, "cmp_w1_v": cmp_w1_v, "cmp_w2_v": cmp_w2_v,
            "nsa_w_q": nsa_w_q, "nsa_w_o": nsa_w_o}


def reference(x, c, mod_w, mod_b, norm_pre_mix, norm_post_mix, norm_pre_ffn, norm_post_ffn,
              ffn_w_in, ffn_w_out, ssm_w_in, ssm_conv_w, ssm_conv_b, ssm_dt_bias, ssm_a_log,
              ssm_d, ssm_norm, ssm_w_out, kv_norm, kv_mod_w, kv_mod_b, w_kv,
              cmp_pos_k, cmp_w1_k, cmp_w2_k, cmp_pos_v, cmp_w1_v, cmp_w2_v, nsa_w_q, nsa_w_o):
    cs = jax.nn.silu(c)
    shared = None
    for i in range(DEPTH):
        sh_m, sc_m, g_m, sh_f, sc_f, g_f = jnp.split(cs @ mod_w[i] + mod_b[i], 6, axis=-1)
        if i == N_A:
            shared = nsa_shared_kv(x, cs, kv_norm, kv_mod_w, kv_mod_b, w_kv,
                                   cmp_pos_k, cmp_w1_k, cmp_w2_k, cmp_pos_v, cmp_w1_v, cmp_w2_v)
        h = adaln(x, norm_pre_mix[i], sh_m, sc_m)
        if i < N_A:
            y = mamba2_mixer(h, ssm_w_in[i], ssm_conv_w[i], ssm_conv_b[i], ssm_dt_bias[i],
                             ssm_a_log[i], ssm_d[i], ssm_norm[i], ssm_w_out[i])
        else:
            y = nsa_mixer(h, nsa_w_q[i - N_A], nsa_w_o[i - N_A], *shared)
        x = x + g_m[:, None, :] * rmsnorm(y, norm_post_mix[i])
        h = adaln(x, norm_pre_ffn[i], sh_f, sc_f)
        x = x + g_f[:, None, :] * rmsnorm(swiglu(h, ffn_w_in[i], ffn_w_out[i]), norm_post_ffn[i])
    return x
```

```python
from contextlib import ExitStack
import numpy as np
import ml_dtypes
import concourse.bass as bass
import concourse.mybir as mybir
from concourse.bass_utils import run_bass_kernel_spmd

F32 = mybir.dt.float32
BF16 = mybir.dt.bfloat16
AF = mybir.ActivationFunctionType
ALU = mybir.AluOpType
AX = mybir.AxisListType

D = 1024
FF = 2816
EPS = 1e-6
NCORES = 8

ENGS = ("pe", "act", "dve", "pool", "sp")
DEBUG_TAGS = False
DEBUG_DUMP = None
SEM_EPOCH = 12000
DMA_SEMS = 8


class Buf:
    __slots__ = ("name", "last_w", "reads")

    def __init__(self, name):
        self.name = name
        self.last_w = None
        self.reads = []


class Op:
    __slots__ = ("eng", "fn", "dma", "deps", "needs_inc", "sem", "val", "tag")

    def __init__(self, eng, fn, dma):
        self.eng = eng
        self.fn = fn
        self.dma = dma
        self.deps = []
        self.needs_inc = False
        self.sem = None
        self.val = 0


class Prog:
    def __init__(self, nc):
        self.nc = nc
        self.es = ExitStack()
        self.ops = {e: [] for e in ENGS}
        self.nbuf = 0
        self.final_ops = []
        self.nsem = 0

    def sb(self, name, shape, dt):
        return self.es.enter_context(self.nc.sbuf_tensor("sb_" + name, list(shape), dt))

    def ps(self, name, shape, dt):
        return self.es.enter_context(self.nc.psum_tensor("ps_" + name, list(shape), dt))

    def buf(self, name=None):
        self.nbuf += 1
        return Buf(name or f"b{self.nbuf}")

    def bufs(self, n):
        return [self.buf() for _ in range(n)]

    def add(self, eng, fn, reads=(), writes=(), dma=False, nosync_same=False):
        op = Op(eng, fn, dma)
        if DEBUG_TAGS:
            import sys as _sys
            op.tag = _sys._getframe(1).f_lineno
        if dma:
            op.needs_inc = True
        deps = {}
        for b in reads:
            if b.last_w is not None:
                deps[id(b.last_w)] = b.last_w
        for b in writes:
            if b.last_w is not None:
                deps[id(b.last_w)] = b.last_w
            for r in b.reads:
                deps[id(r)] = r
        dl = []
        for d in deps.values():
            if nosync_same and d.eng == eng and not d.dma:
                continue
            d.needs_inc = True
            dl.append(d)
        op.deps = dl
        for b in reads:
            b.reads.append(op)
        for b in writes:
            b.last_w = op
            b.reads = []
        self.ops[eng].append(op)
        return op

    def finish_on(self, eng, ops):
        for o in ops:
            o.needs_inc = True
        self.final_ops.append((eng, list(ops)))

    def _newsem(self, tag):
        self.nsem += 1
        return self.es.enter_context(self.nc.semaphore(f"s{self.nsem}_{tag}"))

    def emit(self):
        nc = self.nc
        es = self.es
        for e in ENGS:
            cnt = 0
            cur = None
            dcnt = [0] * DMA_SEMS
            dsems = None
            di = 0
            for op in self.ops[e]:
                if not op.needs_inc:
                    continue
                if op.dma:
                    if dsems is None:
                        dsems = [self._newsem(f"d{e}") for _ in range(DMA_SEMS)]
                    k = di % DMA_SEMS
                    di += 1
                    dcnt[k] += 16
                    op.sem = dsems[k]
                    op.val = dcnt[k]
                else:
                    if cur is None or cnt >= SEM_EPOCH:
                        cur = self._newsem(e)
                        cnt = 0
                    cnt += 1
                    op.sem = cur
                    op.val = cnt
        finals = {e: [] for e in ENGS}
        for e, ops in self.final_ops:
            finals[e].extend(ops)
        block = es.enter_context(nc.Block())

        semname = {}
        for e in ENGS:
            for op in self.ops[e]:
                if op.sem is not None:
                    semname[id(op.sem)] = f"{e}{'d' if op.dma else ''}{id(op.sem) % 1000}"

        def run(e, engobj):
            known = {}
            for op in self.ops[e]:
                need = {}
                for d in op.deps:
                    k = id(d.sem)
                    if k not in need or need[k][1] < d.val:
                        need[k] = (d.sem, d.val)
                wl = []
                for k, (s, v) in need.items():
                    if known.get(k, 0) >= v:
                        continue
                    engobj.wait_ge(s, v)
                    known[k] = v
                    wl.append((semname.get(k, "?"), v))
                ins = op.fn(engobj)
                if op.needs_inc:
                    ins.then_inc(op.sem, 16 if op.dma else 1)
                if DEBUG_DUMP is not None:
                    DEBUG_DUMP.append((e, getattr(op, "tag", None), wl, (semname.get(id(op.sem)), op.val) if op.needs_inc else None))
            for d in finals[e]:
                engobj.wait_ge(d.sem, d.val)

        @block.tensor
        def _(t):
            run("pe", t)

        @block.scalar
        def _(t):
            run("act", t)

        @block.vector
        def _(t):
            run("dve", t)

        @block.gpsimd
        def _(t):
            run("pool", t)

        @block.sync
        def _(t):
            run("sp", t)

    def close(self):
        self.es.close()


class Ctx:
    def __init__(self, P, ident_ap):
        self.P = P
        self.idf = P.sb("idf", [128, 128], F32)
        self.idb = P.sb("idb", [128, 128], BF16)
        self.epsb = P.sb("epsb", [128, 1], F32)
        self.oneb = P.sb("oneb", [128, 1], F32)
        self.b_idf, self.b_idb, self.b_eps, self.b_one = P.bufs(4)
        P.add("sp", lambda e: e.dma_start(out=self.idf[:], in_=ident_ap), writes=[self.b_idf], dma=True)
        P.add("dve", lambda e: e.tensor_copy(out=self.idb[:], in_=self.idf[:]), reads=[self.b_idf], writes=[self.b_idb])
        P.add("pool", lambda e: e.memset(self.epsb[:], EPS), writes=[self.b_eps])
        P.add("pool", lambda e: e.memset(self.oneb[:], 1.0), writes=[self.b_one])


def emit_mods(P, cx, cT_ap, modw_ap, modb_ap, ncols, pbank, b_pbank, tag, nstage=2):
    mods = P.sb(f"mods_{tag}", [128, ncols], F32)
    b_mods = P.buf()
    cs = P.sb(f"cs_{tag}", [128, 8], F32)
    csb = P.sb(f"csb_{tag}", [128, 8, 128], BF16)
    b_cs, b_csb = P.bufs(2)
    stage = [P.sb(f"mst{i}_{tag}", [128, 8, 512], BF16) for i in range(nstage)]
    b_stage = P.bufs(nstage)
    P.add("sp", lambda e: e.dma_start(out=cs[:], in_=cT_ap), writes=[b_cs], dma=True)
    P.add("act", lambda e: e.activation(out=cs[:], in_=cs[:], func=AF.Silu), reads=[b_cs], writes=[b_cs])
    for kc in range(8):
        P.add("dve", lambda e, kc=kc: e.tensor_copy(out=csb[:, kc, :], in_=cs[:, kc:kc + 1].broadcast_to([128, 128])),
              reads=[b_cs], writes=[b_csb])
    P.add("sp", lambda e: e.dma_start(out=mods[:], in_=modb_ap.broadcast_to([128, ncols])), writes=[b_mods], dma=True)
    modw_v = modw_ap.rearrange("(kc p) n -> p kc n", p=128)
    for ci in range(ncols // 512):
        st = stage[ci % nstage]
        bst = b_stage[ci % nstage]
        P.add("pool", lambda e, st=st, ci=ci: e.dma_start(out=st[:], in_=modw_v[:, :, ci * 512:(ci + 1) * 512]), writes=[bst], dma=True)
        for kc in range(8):
            P.add("pe", lambda e, st=st, kc=kc: e.matmul(pbank[:, 0:512], lhsT=csb[:, kc, :], rhs=st[:, kc, :], start=(kc == 0), stop=(kc == 7)),
                  reads=[b_csb, bst], writes=[b_pbank], nosync_same=True)
        P.add("dve", lambda e, ci=ci: e.tensor_tensor(out=mods[:, ci * 512:(ci + 1) * 512], in0=pbank[:, 0:512], in1=mods[:, ci * 512:(ci + 1) * 512], op=ALU.add),
              reads=[b_pbank, b_mods], writes=[b_mods])
    return mods, b_mods


def emit_rstd(P, cx, ss_ap, rs_tmp_ap, rs_out_ap, b_ss, b_rs, n):
    P.add("act", lambda e: e.activation(out=rs_tmp_ap, in_=ss_ap, func=AF.Sqrt, scale=1.0 / n, bias=cx.epsb[:]),
          reads=[b_ss, cx.b_eps], writes=[b_rs])
    P.add("dve", lambda e: e.reciprocal(out=rs_out_ap, in_=rs_tmp_ap), reads=[b_rs], writes=[b_rs])


class AdaLN:
    def __init__(self, P, cx, A_ap, sh_ap, b_vec, tp, b_tp, tag):
        self.P, self.cx = P, cx
        self.A_ap, self.sh_ap, self.b_vec = A_ap, sh_ap, b_vec
        self.tp, self.b_tp = tp, b_tp
        self.hf = P.sb(f"hf_{tag}", [128, D], F32)
        self.hb = P.sb(f"hb_{tag}", [128, D], BF16)
        self.junk = P.sb(f"junk_{tag}", [128, D], BF16)
        self.ss = P.sb(f"ss_{tag}", [128, 2], F32)
        self.rs = P.sb(f"rs_{tag}", [128, 2], F32)
        self.b_hf, self.b_hb, self.b_junk, self.b_ss, self.b_rs = P.bufs(5)

    def emit(self, x_ap, b_x, hT_dst, b_hT):
        P, cx = self.P, self.cx
        P.add("act", lambda e: e.activation(out=self.junk[:], in_=x_ap, func=AF.Square, accum_out=self.ss[:, 0:1]),
              reads=[b_x], writes=[self.b_junk, self.b_ss])
        emit_rstd(P, cx, self.ss[:, 0:1], self.rs[:, 0:1], self.rs[:, 1:2], self.b_ss, self.b_rs, D)
        P.add("dve", lambda e: e.scalar_tensor_tensor(out=self.hf[:], in0=x_ap, scalar=self.rs[:, 1:2], in1=self.A_ap, op0=ALU.mult, op1=ALU.mult),
              reads=[b_x, self.b_rs, self.b_vec], writes=[self.b_hf])
        P.add("pool", lambda e: e.tensor_tensor(out=self.hb[:], in0=self.hf[:], in1=self.sh_ap, op=ALU.add),
              reads=[self.b_hf, self.b_vec], writes=[self.b_hb])
        for kc in range(8):
            P.add("pe", lambda e, kc=kc: e.transpose(out=self.tp[:, kc, :], in_=self.hb[:, kc * 128:(kc + 1) * 128], identity=cx.idb[:]),
                  reads=[self.b_hb, cx.b_idb], writes=[self.b_tp], nosync_same=True)
        P.add("act", lambda e: e.copy(out=hT_dst, in_=self.tp[:]), reads=[self.b_tp], writes=[b_hT])


NH = 16
HP = 64
NST = 128
WCOLS = 2576


class _Stop(Exception):
    pass


def build_mamba(S, stage=99):
    def chk(n):
        if stage <= n:
            raise _Stop()
    nc = bass.Bass("TRN2", target_bir_lowering=False)
    dram = lambda n, s, dt=F32, kind="ExternalInput": nc.dram_tensor(n, list(s), dt, kind=kind).ap()
    x = dram("x", [S, D])
    cT = dram("cT", [128, 8])
    ident = dram("ident", [128, 128])
    modw = dram("modw", [D, 2048])
    modb = dram("modb", [1, 2048])
    gpre = dram("gpre", [1, D])
    w_in = dram("w_in", [D, WCOLS])
    convw = dram("convw", [128, 12, 4])
    convb = dram("convb", [128, 12])
    hvec = dram("hvec", [3, NH])
    normg = dram("normg", [1, 1024])
    tri_d = dram("tri", [128, 128])
    sel_d = dram("sel", [16, 16 * 128])
    onelast_d = dram("onelast", [128, 128])
    yn = dram("yn", [S, 1024], BF16, kind="ExternalOutput")

    P = Prog(nc)
    cx = Ctx(P, ident)
    SC = 512
    NSC = S // SC
    tp = P.ps("tp", [128, 8, 128], BF16); b_tp = P.buf()
    pA = P.ps("pA", [128, 512], F32); b_pA = P.buf()
    pB = P.ps("pB", [128, 512], F32); b_pB = P.buf()
    psm = P.ps("psm", [128, 512], F32)
    b_psm = P.buf()
    b_pdt = b_pacs = b_pdl = b_pacsT = b_pcb = b_pbt = b_psm
    pseg = [P.ps(f"pseg{i}", [128, 512], F32) for i in range(2)]
    b_pseg0, b_pseg1 = P.bufs(2)
    pyi = P.ps("pyi", [128, 512], F32); b_pyi = P.buf()
    pyn = P.ps("pyn", [128, 512], F32); b_pyn = P.buf()
    psn = pyi; b_psn = b_pyi

    w = P.sb("w", [128, 8, WCOLS], BF16); b_w = P.buf()
    w_v = w_in.rearrange("(kc p) n -> p kc n", p=128)
    for kc in range(8):
        P.add("pool", lambda e, kc=kc: e.dma_start(out=w[:, kc, :], in_=w_v[:, kc, :]), writes=[b_w], dma=True)
    mods, b_mods = emit_mods(P, cx, cT, modw, modb, 2048, pA, b_pA, "m")
    gp = P.sb("gp", [128, D], F32); b_gp = P.buf()
    P.add("sp", lambda e: e.dma_start(out=gp[:], in_=gpre.broadcast_to([128, D])), writes=[b_gp], dma=True)
    P.add("dve", lambda e: e.scalar_tensor_tensor(out=gp[:], in0=mods[:, 1024:2048], scalar=1.0, in1=gp[:], op0=ALU.add, op1=ALU.mult),
          reads=[b_mods, b_gp], writes=[b_gp])
    ng = P.sb("ng", [128, 1024], F32); b_ng = P.buf()
    P.add("sp", lambda e: e.dma_start(out=ng[:], in_=normg.broadcast_to([128, 1024])), writes=[b_ng], dma=True)
    cw = P.sb("cw", [128, 12, 4], F32); cb_ = P.sb("cb", [128, 12], F32); b_cw = P.buf()
    P.add("sp", lambda e: e.dma_start(out=cw[:], in_=convw), writes=[b_cw], dma=True)
    P.add("sp", lambda e: e.dma_start(out=cb_[:], in_=convb), writes=[b_cw], dma=True)
    hv = P.sb("hv", [128, 3, NH], F32); b_hv = P.buf()
    for i in range(3):
        P.add("sp", lambda e, i=i: e.dma_start(out=hv[:, i, :], in_=hvec[i:i + 1, :].broadcast_to([128, NH])), writes=[b_hv], dma=True)
    Ab = P.sb("Ab", [128, NH], F32); b_Ab = P.buf()
    P.add("act", lambda e: e.activation(out=Ab[:], in_=hv[:, 1, :], func=AF.Exp), reads=[b_hv], writes=[b_Ab])
    P.add("dve", lambda e: e.tensor_scalar(out=Ab[:], in0=Ab[:], scalar1=-1.0, scalar2=None, op0=ALU.mult), reads=[b_Ab], writes=[b_Ab])
    tri = P.sb("tri", [128, 128], F32); onel = P.sb("onel", [128, 128], F32); sel = P.sb("sel", [16, 16 * 128], F32)
    b_cst = P.buf()
    P.add("sp", lambda e: e.dma_start(out=tri[:], in_=tri_d), writes=[b_cst], dma=True)
    P.add("sp", lambda e: e.dma_start(out=onel[:], in_=onelast_d), writes=[b_cst], dma=True)
    P.add("sp", lambda e: e.dma_start(out=sel[:], in_=sel_d), writes=[b_cst], dma=True)

    ada = AdaLN(P, cx, gp[:], mods[:, 0:1024], b_gp, tp, b_tp, "m")
    ada_b_extra = b_mods

    xt = [P.sb(f"xt{i}", [128, D], F32) for i in range(2)]; b_xt = P.bufs(2)
    hT = P.sb("hT", [128, 8, SC], BF16); b_hT = P.buf()
    cin = P.sb("cin", [128, 12, SC + 3], F32); b_cin = [P.buf() for _ in range(12)]; b_halo = P.buf()
    ctmp = [P.sb(f"ctmp{i}", [128, SC], F32) for i in range(2)]; b_ctmp = P.bufs(2)
    xsT = P.sb("xsT", [128, 8, SC], BF16); b_xsT = [P.buf() for _ in range(8)]
    BCT = P.sb("BCT", [128, 4, SC], BF16); b_BCT = [P.buf() for _ in range(4)]
    xs = P.sb("xs", [128, 4, 1024], BF16); b_xs = P.bufs(4)
    sz = P.sb("sz", [128, 4, 1024], BF16); b_sz = P.bufs(4)
    dtt = P.sb("dtt", [128, 4, 8, NH], F32)
    b_dtt = P.bufs(4)
    acsT = P.sb("acsT", [16, 4, 2, 128], F32); b_acsT = P.bufs(4)
    Xdt = P.sb("Xdt", [128, 1024], BF16); b_Xdt = P.buf()
    Xw = P.sb("Xw", [128, 1024], BF16); b_Xw = P.buf()
    cbm = P.sb("cbm", [128, 128], F32); b_cbm = P.buf()
    dec = [P.sb(f"dec{i}", [128, 128], F32) for i in range(2)]; b_dec = P.bufs(2)
    MT = [P.sb(f"MT{i}", [128, 128], BF16) for i in range(2)]; b_MT = P.bufs(2)
    t1 = P.sb("t1", [128, 512], F32); b_t1 = P.buf()
    yt = P.sb("yt", [128, 1024], F32); b_yt = P.buf()
    St = P.sb("St", [128, 2, 512], F32); b_St = P.bufs(2)
    Sbf = P.sb("Sbf", [128, 2, 512], BF16); b_Sbf = P.bufs(2)
    Btok = P.sb("Btok", [128, 128], BF16); b_Btok = P.buf()
    gt = P.sb("gt", [128, 1024], F32); b_gt = P.buf()
    gss = P.sb("gss", [128, 4], F32); b_gss = P.buf()
    grs = P.sb("grs", [128, 4], F32); b_grs = P.buf()
    gjunk = P.sb("gjunk", [128, 512], BF16); b_gjunk = P.buf()
    ob = [P.sb(f"ob{i}", [128, 1024], BF16) for i in range(2)]; b_ob = P.bufs(2)

    P.add("pool", lambda e: e.memset(cin[:, :, 0:3], 0.0), writes=b_cin + [b_halo])
    P.add("pool", lambda e: e.memset(St[:], 0.0), writes=b_St)
    P.add("pool", lambda e: e.memset(Sbf[:], 0.0), writes=b_Sbf)
    pbt_bf = psm[:, 320:384].bitcast(BF16)

    outs = []
    banks = [(pA, b_pA), (pB, b_pB)]
    bi = 0
    try:
      for sc in range(NSC):
          chk(0)
          for j in range(4):
              r0 = sc * SC + j * 128
              xj = xt[j % 2]; bxj = b_xt[j % 2]
              P.add("sp", lambda e, xj=xj, r0=r0: e.dma_start(out=xj[:], in_=x[r0:r0 + 128, :]), writes=[bxj], dma=True)
              ada.emit(xj[:], bxj, hT[:, :, j * 128:(j + 1) * 128], b_hT)
          chk(1)
          for cc in range(12):
              pb, bpb = banks[bi % 2]; bi += 1
              col = 1024 + cc * 128
              for kc in range(8):
                  P.add("pe", lambda e, kc=kc, col=col, pb=pb: e.matmul(pb[:, :], lhsT=w[:, kc, col:col + 128], rhs=hT[:, kc, :], start=(kc == 0), stop=(kc == 7)),
                        reads=[b_w, b_hT], writes=[bpb], nosync_same=True)
              P.add("act", lambda e, cc=cc, pb=pb: e.copy(out=cin[:, cc, 3:SC + 3], in_=pb[:, :]), reads=[bpb, b_halo], writes=[b_cin[cc]])
              ct = ctmp[cc % 2]; bct = b_ctmp[cc % 2]
              P.add("act", lambda e, cc=cc, ct=ct: e.activation(out=ct[:], in_=cin[:, cc, 3:SC + 3], func=AF.Identity, scale=cw[:, cc, 3:4], bias=cb_[:, cc:cc + 1]),
                    reads=[b_cin[cc], b_cw], writes=[bct])
              for k in range(3):
                  P.add("dve", lambda e, cc=cc, ct=ct, k=k: e.scalar_tensor_tensor(out=ct[:], in0=cin[:, cc, k:k + SC], scalar=cw[:, cc, k:k + 1], in1=ct[:], op0=ALU.mult, op1=ALU.add),
                        reads=[b_cin[cc], b_cw, bct], writes=[bct])
              if cc < 8:
                  P.add("act", lambda e, cc=cc, ct=ct: e.activation(out=xsT[:, cc, :], in_=ct[:], func=AF.Silu), reads=[bct], writes=[b_xsT[cc]])
              else:
                  P.add("act", lambda e, cc=cc, ct=ct: e.activation(out=BCT[:, cc - 8, :], in_=ct[:], func=AF.Silu), reads=[bct], writes=[b_BCT[cc - 8]])
          P.add("pool", lambda e: e.tensor_copy(out=cin[:, :, 0:3], in_=cin[:, :, SC:SC + 3]), reads=b_cin, writes=b_cin + [b_halo])
          chk(2)
          for j in range(4):
              for cc in range(8):
                  P.add("pe", lambda e, cc=cc, j=j: e.transpose(out=tp[:, cc, :], in_=xsT[:, cc, j * 128:(j + 1) * 128], identity=cx.idb[:]),
                        reads=[b_xsT[cc], cx.b_idb], writes=[b_tp], nosync_same=True)
              P.add("act", lambda e, j=j: e.copy(out=xs[:, j, :], in_=tp[:].rearrange("p a b -> p (a b)")), reads=[b_tp], writes=[b_xs[j]])
          chk(3)
          for j in range(4):
              for half in range(2):
                  pb, bpb = banks[bi % 2]; bi += 1
                  for kc in range(8):
                      P.add("pe", lambda e, kc=kc, j=j, half=half, pb=pb: e.matmul(pb[:, :], lhsT=hT[:, kc, j * 128:(j + 1) * 128], rhs=w[:, kc, half * 512:(half + 1) * 512], start=(kc == 0), stop=(kc == 7)),
                            reads=[b_w, b_hT], writes=[bpb], nosync_same=True)
                  P.add("act", lambda e, j=j, half=half, pb=pb: e.activation(out=sz[:, j, half * 512:(half + 1) * 512], in_=pb[:, :], func=AF.Silu),
                        reads=[bpb], writes=[b_sz[j]])
              chk(3.2)
              dj = dtt[:, j]
              bd = b_dtt[j]
              for kc in range(8):
                  P.add("pe", lambda e, kc=kc, j=j: e.matmul(psm[:, 0:16], lhsT=hT[:, kc, j * 128:(j + 1) * 128], rhs=w[:, kc, 2560:2576], start=(kc == 0), stop=(kc == 7)),
                        reads=[b_w, b_hT], writes=[b_pdt], nosync_same=True)
              P.add("dve", lambda e, dj=dj: e.tensor_tensor(out=dj[:, 7, :], in0=psm[:, 0:16], in1=hv[:, 0, :], op=ALU.add), reads=[b_pdt, b_hv], writes=[bd])
              P.add("act", lambda e, dj=dj: e.activation(out=dj[:, 7, :], in_=dj[:, 7, :], func=AF.Exp), reads=[bd], writes=[bd])
              P.add("act", lambda e, dj=dj: e.activation(out=dj[:, 0, :], in_=dj[:, 7, :], func=AF.Ln, bias=cx.oneb[:]), reads=[bd, cx.b_one], writes=[bd])
              P.add("dve", lambda e, dj=dj: e.tensor_tensor(out=dj[:, 1, :], in0=dj[:, 0, :], in1=Ab[:], op=ALU.mult), reads=[bd, b_Ab], writes=[bd])
              chk(3.4)
              P.add("pe", lambda e, dj=dj: e.matmul(psm[:, 16:32], lhsT=tri[:], rhs=dj[:, 1, :], start=True, stop=True), reads=[bd, b_cst], writes=[b_pacs])
              P.add("dve", lambda e, dj=dj: e.tensor_copy(out=dj[:, 2, :], in_=psm[:, 16:32]), reads=[b_pacs], writes=[bd])
              chk(3.6)
              P.add("pe", lambda e, dj=dj: e.matmul(psm[0:16, 64:192], lhsT=dj[:, 1, :], rhs=tri[:], start=True, stop=True), reads=[bd, b_cst], writes=[b_pacsT])
              P.add("dve", lambda e, j=j: e.tensor_copy(out=acsT[:, j, 0, :], in_=psm[0:16, 64:192]), reads=[b_pacsT], writes=[b_acsT[j]])
              P.add("dve", lambda e, j=j: e.tensor_scalar(out=acsT[:, j, 1, :], in0=psm[0:16, 64:192], scalar1=-1.0, scalar2=None, op0=ALU.mult), reads=[b_pacsT], writes=[b_acsT[j]])
              chk(3.8)
              P.add("pe", lambda e, dj=dj: e.matmul(psm[:, 32:48], lhsT=onel[:], rhs=dj[:, 2, :], start=True, stop=True), reads=[bd, b_cst], writes=[b_pdl])
              P.add("dve", lambda e, dj=dj: e.tensor_copy(out=dj[:, 4, :], in_=psm[:, 32:48]), reads=[b_pdl], writes=[bd])
              chk(3.85)
              P.add("act", lambda e, dj=dj: e.activation(out=dj[:, 3, :], in_=dj[:, 2, :], func=AF.Exp), reads=[bd], writes=[bd])
              P.add("act", lambda e, dj=dj: e.activation(out=dj[:, 5, :], in_=dj[:, 4, :], func=AF.Exp), reads=[bd], writes=[bd])
              chk(3.87)
              P.add("dve", lambda e, dj=dj: e.tensor_tensor(out=dj[:, 7, :], in0=dj[:, 4, :], in1=dj[:, 2, :], op=ALU.subtract), reads=[bd], writes=[bd])
              P.add("act", lambda e, dj=dj: e.activation(out=dj[:, 7, :], in_=dj[:, 7, :], func=AF.Exp), reads=[bd], writes=[bd])
              P.add("dve", lambda e, dj=dj: e.tensor_tensor(out=dj[:, 6, :], in0=dj[:, 7, :], in1=dj[:, 0, :], op=ALU.mult), reads=[bd], writes=[bd])
              chk(3.9)
          chk(4)
          for j in range(4):
              dj = dtt[:, j]
              bd = b_dtt[j]
              tsl = slice(j * 128, (j + 1) * 128)
              xs3 = xs[:, j, :].rearrange("p (h d) -> p h d", h=NH)
              P.add("dve", lambda e, dj=dj, xs3=xs3: e.tensor_tensor(out=Xdt[:].rearrange("p (h d) -> p h d", h=NH), in0=xs3, in1=dj[:, 0, :].unsqueeze(2).broadcast_to([128, NH, HP]), op=ALU.mult),
                    reads=[b_xs[j], bd], writes=[b_Xdt])
              P.add("pool", lambda e, dj=dj, xs3=xs3: e.tensor_tensor(out=Xw[:].rearrange("p (h d) -> p h d", h=NH), in0=xs3, in1=dj[:, 6, :].unsqueeze(2).broadcast_to([128, NH, HP]), op=ALU.mult),
                    reads=[b_xs[j], bd], writes=[b_Xw])
              for g in range(2):
                  P.add("pe", lambda e, g=g, tsl=tsl: e.matmul(psm[:, 192:320], lhsT=BCT[:, g, tsl], rhs=BCT[:, 2 + g, tsl], start=True, stop=True),
                        reads=[b_BCT[g], b_BCT[2 + g]], writes=[b_pcb])
                  P.add("dve", lambda e: e.tensor_tensor(out=cbm[:], in0=psm[:, 192:320], in1=tri[:], op=ALU.mult), reads=[b_pcb, b_cst], writes=[b_cbm])
                  P.add("pe", lambda e, g=g, tsl=tsl: e.matmul(pyi[:, :], lhsT=BCT[:, 2 + g, tsl], rhs=Sbf[:, g, :], start=True, stop=True),
                        reads=[b_BCT[2 + g], b_Sbf[g]], writes=[b_pyi])
                  P.add("dve", lambda e, g=g, dj=dj: e.tensor_tensor(out=t1[:].rearrange("p (h d) -> p h d", h=8), in0=pyi[:, :].rearrange("p (h d) -> p h d", h=8),
                                                                in1=dj[:, 3, g * 8:(g + 1) * 8].unsqueeze(2).broadcast_to([128, 8, HP]), op=ALU.mult),
                        reads=[b_pyi, bd], writes=[b_t1])
                  for hh in range(8):
                      h = g * 8 + hh
                      k2 = hh % 2
                      bseg = (b_pseg0, b_pseg1)[k2]
                      P.add("pe", lambda e, h=h, j=j, k2=k2: e.matmul(pseg[k2][:, 0:128], lhsT=sel[:, h * 128:(h + 1) * 128], rhs=acsT[:, j, 0, :], start=True, stop=False),
                            reads=[b_cst, b_acsT[j]], writes=[bseg], nosync_same=True)
                      P.add("pe", lambda e, h=h, j=j, k2=k2: e.matmul(pseg[k2][:, 0:128], lhsT=acsT[:, j, 1, :], rhs=sel[:, h * 128:(h + 1) * 128], start=False, stop=True),
                            reads=[b_cst, b_acsT[j]], writes=[bseg], nosync_same=True)
                      P.add("act", lambda e, k2=k2: e.activation(out=dec[k2][:], in_=pseg[k2][:, 0:128], func=AF.Exp), reads=[bseg], writes=[b_dec[k2]])
                      P.add("dve", lambda e, k2=k2: e.scalar_tensor_tensor(out=MT[k2][:], in0=dec[k2][:], scalar=1e30, in1=cbm[:], op0=ALU.min, op1=ALU.mult),
                            reads=[b_dec[k2], b_cbm], writes=[b_MT[k2]])
                      P.add("pe", lambda e, h=h, hh=hh, k2=k2: e.matmul(pyn[:, hh * 64:(hh + 1) * 64], lhsT=MT[k2][:], rhs=Xdt[:, h * 64:(h + 1) * 64], start=True, stop=True),
                            reads=[b_MT[k2], b_Xdt], writes=[b_pyn], nosync_same=True)
                  P.add("dve", lambda e, g=g: e.tensor_tensor(out=yt[:, g * 512:(g + 1) * 512], in0=t1[:], in1=pyn[:, :], op=ALU.add),
                        reads=[b_t1, b_pyn], writes=[b_yt])
                  P.add("pe", lambda e, g=g, tsl=tsl: e.transpose(out=pbt_bf[:, 0:128], in_=BCT[:, g, tsl], identity=cx.idb[:]),
                        reads=[b_BCT[g], cx.b_idb], writes=[b_pbt])
                  P.add("act", lambda e: e.copy(out=Btok[:], in_=pbt_bf[:, 0:128]), reads=[b_pbt], writes=[b_Btok])
                  P.add("pe", lambda e, g=g: e.matmul(psn[:, :], lhsT=Btok[:], rhs=Xw[:, g * 512:(g + 1) * 512], start=True, stop=True),
                        reads=[b_Btok, b_Xw], writes=[b_psn])
                  P.add("pool", lambda e, g=g, dj=dj: e.tensor_tensor(out=St[:, g, :].rearrange("p (h d) -> p h d", h=8), in0=St[:, g, :].rearrange("p (h d) -> p h d", h=8),
                                                                 in1=dj[:, 5, g * 8:(g + 1) * 8].unsqueeze(2).broadcast_to([128, 8, HP]), op=ALU.mult),
                        reads=[b_St[g], bd], writes=[b_St[g]])
                  P.add("dve", lambda e, g=g: e.tensor_tensor(out=St[:, g, :], in0=St[:, g, :], in1=psn[:, :], op=ALU.add),
                        reads=[b_St[g], b_psn], writes=[b_St[g]])
                  P.add("act", lambda e, g=g: e.copy(out=Sbf[:, g, :], in_=St[:, g, :]), reads=[b_St[g]], writes=[b_Sbf[g]])
              chk(5)
              P.add("pool", lambda e, xs3=xs3: e.tensor_tensor(out=gt[:].rearrange("p (h d) -> p h d", h=NH), in0=xs3, in1=hv[:, 2, :].unsqueeze(2).broadcast_to([128, NH, HP]), op=ALU.mult),
                    reads=[b_xs[j], b_hv], writes=[b_gt])
              P.add("dve", lambda e: e.tensor_tensor(out=gt[:], in0=gt[:], in1=yt[:], op=ALU.add), reads=[b_gt, b_yt], writes=[b_gt])
              P.add("dve", lambda e, j=j: e.tensor_tensor(out=gt[:], in0=gt[:], in1=sz[:, j, :], op=ALU.mult), reads=[b_gt, b_sz[j]], writes=[b_gt])
              for g in range(2):
                  P.add("act", lambda e, g=g: e.activation(out=gjunk[:], in_=gt[:, g * 512:(g + 1) * 512], func=AF.Square, accum_out=gss[:, g:g + 1]),
                        reads=[b_gt], writes=[b_gjunk, b_gss])
              P.add("act", lambda e: e.activation(out=grs[:, 0:2], in_=gss[:, 0:2], func=AF.Sqrt, scale=1.0 / 512, bias=cx.epsb[:]),
                    reads=[b_gss, cx.b_eps], writes=[b_grs])
              P.add("dve", lambda e: e.reciprocal(out=grs[:, 2:4], in_=grs[:, 0:2]), reads=[b_grs], writes=[b_grs])
              obj = ob[j % 2]; bob = b_ob[j % 2]
              for g in range(2):
                  P.add("dve", lambda e, g=g, obj=obj: e.scalar_tensor_tensor(out=obj[:, g * 512:(g + 1) * 512], in0=gt[:, g * 512:(g + 1) * 512], scalar=grs[:, 2 + g:3 + g],
                                                                      in1=ng[:, g * 512:(g + 1) * 512], op0=ALU.mult, op1=ALU.mult),
                        reads=[b_gt, b_grs, b_ng], writes=[bob])
              r0 = sc * SC + j * 128
              outs.append(P.add("sp", lambda e, obj=obj, r0=r0: e.dma_start(out=yn[r0:r0 + 128, :], in_=obj[:]), reads=[bob], dma=True))
    except _Stop:
        pass
    P.finish_on("sp", outs)
    P.emit()
    P.close()
    return nc


def mamba_consts():
    tri = np.triu(np.ones((128, 128), np.float32))
    sel = np.zeros((16, 16, 128), np.float32)
    for h in range(16):
        sel[h, h, :] = 1.0
    onelast = np.zeros((128, 128), np.float32)
    onelast[127, :] = 1.0
    return dict(tri=tri, sel=sel.reshape(16, 16 * 128), onelast=onelast, ident=np.eye(128, dtype=np.float32))


def mamba_inputs(x_b, c_b, mod_w_i, mod_b_i, gpre_i, ssm_w_in_i, conv_w_i, conv_b_i, dt_bias_i, a_log_i, d_i, norm_i, hp):
    z_cols = np.arange(hp * 1024, hp * 1024 + 1024)
    x_cols = 2048 + np.arange(hp * 1024, hp * 1024 + 1024)
    B_cols = 4096 + np.arange(hp * 256, hp * 256 + 256)
    C_cols = 4096 + 512 + np.arange(hp * 256, hp * 256 + 256)
    dt_cols = 5120 + np.arange(hp * 16, hp * 16 + 16)
    cols = np.concatenate([z_cols, x_cols, B_cols, C_cols, dt_cols])
    conv_ch = np.concatenate([x_cols, B_cols, C_cols]) - 2048
    cwc = conv_w_i[:, conv_ch]
    convw = np.ascontiguousarray(cwc.T.reshape(12, 128, 4).transpose(1, 0, 2))
    convb = np.ascontiguousarray(conv_b_i[conv_ch].reshape(12, 128).T)
    hs = slice(hp * 16, hp * 16 + 16)
    d = dict(
        x=np.ascontiguousarray(x_b),
        cT=np.ascontiguousarray(c_b.reshape(8, 128).T),
        modw=np.ascontiguousarray(mod_w_i[:, 0:2048]),
        modb=np.ascontiguousarray(mod_b_i[None, 0:2048]),
        gpre=np.ascontiguousarray(gpre_i[None, :]),
        w_in=np.ascontiguousarray(ssm_w_in_i[:, cols]),
        convw=convw, convb=convb,
        hvec=np.ascontiguousarray(np.stack([dt_bias_i[hs], a_log_i[hs], d_i[hs]])),
        normg=np.ascontiguousarray(norm_i[None, hp * 1024: hp * 1024 + 1024]),
    )
    d.update(mamba_consts())
    return d


def build_f(T, KM):
    nc = bass.Bass("TRN2", target_bir_lowering=False)
    dram = lambda n, s, dt=F32, kind="ExternalInput": nc.dram_tensor(n, list(s), dt, kind=kind).ap()
    x = dram("x", [T, D])
    ym = dram("ym", [T, KM], BF16)
    cT = dram("cT", [128, 8])
    ident = dram("ident", [128, 128])
    modw = dram("modw", [D, 4096])
    modb = dram("modb", [1, 4096])
    gvec = dram("gvec", [3, D])
    wo = dram("wo", [KM, D])
    w_in = dram("w_in", [D, 2 * FF])
    w_out = dram("w_out", [FF, D])
    xmid = dram("xmid", [T, D], F32, kind="Internal")
    y = dram("y", [T, D], F32, kind="ExternalOutput")

    P = Prog(nc)
    cx = Ctx(P, ident)
    KC = D // 128
    FC = FF // 128
    KMC = KM // 128
    ST = 256
    JT = 2
    NSUP = T // ST
    tp = P.ps("tp", [128, 8, 128], BF16); b_tp = P.buf()
    pab = [P.ps(f"pab{i}", [128, 2, ST], F32) for i in range(2)]; b_pab = P.bufs(2)
    po = [P.ps(f"po{i}", [128, D], F32) for i in range(2)]; b_po = P.bufs(2)

    arena = P.sb("arena", [128, KC * 2 * FF + FC * D], BF16)
    w1 = arena[:, 0:KC * 2 * FF].rearrange("p (kc n) -> p kc n", kc=KC)
    w2 = arena[:, KC * 2 * FF:].rearrange("p (fc n) -> p fc n", fc=FC)
    wov = arena[:, KC * 2 * FF:KC * 2 * FF + KMC * D].rearrange("p (kc n) -> p kc n", kc=KMC)
    b_w1, b_w2 = P.bufs(2)
    wo_v = wo.rearrange("(kc p) n -> p kc n", p=128)
    for kc in range(0, KMC, 4):
        P.add("pool", lambda e, kc=kc: e.dma_start(out=wov[:, kc:kc + 4, :], in_=wo_v[:, kc:kc + 4, :]), writes=[b_w2], dma=True)
    mods, b_mods = emit_mods(P, cx, cT, modw, modb, 4096, po[0][:, 0:512], b_po[0], "f", nstage=1)
    tmp = P.sb("tmp", [128, D], F32); b_tmp = P.buf()
    P.add("sp", lambda e: e.dma_start(out=tmp[:], in_=gvec[0:1, :].broadcast_to([128, D])), writes=[b_tmp], dma=True)
    P.add("dve", lambda e: e.tensor_tensor(out=mods[:, 0:1024], in0=mods[:, 0:1024], in1=tmp[:], op=ALU.mult), reads=[b_mods, b_tmp], writes=[b_mods])
    P.add("sp", lambda e: e.dma_start(out=tmp[:], in_=gvec[1:2, :].broadcast_to([128, D])), writes=[b_tmp], dma=True)
    P.add("dve", lambda e: e.scalar_tensor_tensor(out=mods[:, 2048:3072], in0=mods[:, 2048:3072], scalar=1.0, in1=tmp[:], op0=ALU.add, op1=ALU.mult),
          reads=[b_mods, b_tmp], writes=[b_mods])
    P.add("sp", lambda e: e.dma_start(out=tmp[:], in_=gvec[2:3, :].broadcast_to([128, D])), writes=[b_tmp], dma=True)
    P.add("dve", lambda e: e.tensor_tensor(out=mods[:, 3072:4096], in0=mods[:, 3072:4096], in1=tmp[:], op=ALU.mult), reads=[b_mods, b_tmp], writes=[b_mods])

    ada = AdaLN(P, cx, mods[:, 2048:3072], mods[:, 1024:2048], b_mods, tp, b_tp, "f")
    xt = [P.sb(f"xt{i}", [128, JT, D], F32) for i in range(2)]
    b_xt = [P.bufs(JT) for _ in range(2)]
    hT = P.sb("hT", [128, KC, ST], BF16); b_hT = P.buf()
    uTa = P.sb("uT", [128, FC * ST], BF16); b_uT = P.buf()
    uT = uTa[:].rearrange("p (fc t) -> p fc t", fc=FC)
    ymt = uTa[:, 0:KM]
    yT = uTa[:, 2048:2048 + KMC * 128].rearrange("p (kc t) -> p kc t", kc=KMC)
    b_ymt = P.buf(); b_yT = P.buf()
    sg = [P.sb(f"sg{i}", [128, ST], F32) for i in range(2)]; b_sg = P.bufs(2)
    ss2 = P.sb("ss2", [128, 2], F32); b_ss2 = P.buf()
    rs2 = P.sb("rs2", [128, 2], F32); b_rs2 = P.buf()
    b_xm = [P.buf() for _ in range(T // 128)]

    def post(pj, bpj, G_ap, xs_ap, bx):
        P.add("act", lambda e: e.activation(out=ada.junk[:], in_=pj[:], func=AF.Square, accum_out=ss2[:, 0:1]),
              reads=[bpj], writes=[ada.b_junk, b_ss2])
        emit_rstd(P, cx, ss2[:, 0:1], rs2[:, 0:1], rs2[:, 1:2], b_ss2, b_rs2, D)
        P.add("dve", lambda e: e.scalar_tensor_tensor(out=tmp[:], in0=pj[:], scalar=rs2[:, 1:2], in1=G_ap, op0=ALU.mult, op1=ALU.mult),
              reads=[bpj, b_rs2, b_mods], writes=[b_tmp])
        P.add("pool", lambda e: e.tensor_tensor(out=xs_ap, in0=tmp[:], in1=xs_ap, op=ALU.add), reads=[b_tmp, bx], writes=[bx])

    for ti in range(T // 128):
        r0 = ti * 128
        xs = xt[ti % 2]; bx = b_xt[ti % 2][0]
        P.add("sp", lambda e, xs=xs, r0=r0: e.dma_start(out=xs[:, 0, :], in_=x[r0:r0 + 128, :]), writes=[bx], dma=True)
        P.add("sp", lambda e, r0=r0: e.dma_start(out=ymt, in_=ym[r0:r0 + 128, :]), writes=[b_ymt, b_uT], dma=True)
        for k0 in range(0, KMC, 8):
            for kc in range(k0, k0 + 8):
                P.add("pe", lambda e, kc=kc, k0=k0: e.transpose(out=tp[:, kc - k0, :], in_=ymt[:, kc * 128:(kc + 1) * 128], identity=cx.idb[:]),
                      reads=[b_ymt, cx.b_idb], writes=[b_tp], nosync_same=True)
            P.add("act", lambda e, k0=k0: e.copy(out=yT[:, k0:k0 + 8, :], in_=tp[:]), reads=[b_tp], writes=[b_yT, b_uT])
        pj = po[ti % 2]; bpj = b_po[ti % 2]
        for nb in range(2):
            for kc in range(KMC):
                P.add("pe", lambda e, kc=kc, nb=nb, pj=pj: e.matmul(pj[:, nb * 512:(nb + 1) * 512], lhsT=yT[:, kc, :], rhs=wov[:, kc, nb * 512:(nb + 1) * 512],
                                                             start=(kc == 0), stop=(kc == KMC - 1)),
                      reads=[b_yT, b_w2], writes=[bpj], nosync_same=True)
        post(pj, bpj, mods[:, 0:1024], xs[:, 0, :], bx)
        P.add("sp", lambda e, xs=xs, r0=r0: e.dma_start(out=xmid[r0:r0 + 128, :], in_=xs[:, 0, :]), reads=[bx], writes=[b_xm[ti]], dma=True)

    w_in_v = w_in.rearrange("(kc p) n -> p kc n", p=128)
    for kc in range(KC):
        P.add("pool", lambda e, kc=kc: e.dma_start(out=w1[:, kc, :], in_=w_in_v[:, kc, :]), writes=[b_w1], dma=True)
    w_out_v = w_out.rearrange("(fc p) n -> p fc n", p=128)
    for fc in range(0, FC, 2):
        P.add("pool", lambda e, fc=fc: e.dma_start(out=w2[:, fc:fc + 2, :], in_=w_out_v[:, fc:fc + 2, :]), writes=[b_w2], dma=True)
    outs = []
    for s in range(NSUP):
        xs = xt[s % 2]; bx = b_xt[s % 2]
        for j in range(JT):
            ti = s * JT + j
            r0 = ti * 128
            P.add("sp", lambda e, j=j, r0=r0, xs=xs: e.dma_start(out=xs[:, j, :], in_=xmid[r0:r0 + 128, :]), reads=[b_xm[ti]], writes=[bx[j]], dma=True)
            ada.emit(xs[:, j, :], bx[j], hT[:, :, j * 128:(j + 1) * 128], b_hT)
        for fc in range(FC):
            pb = pab[fc % 2]; bpb = b_pab[fc % 2]
            for part in range(2):
                col = part * FF + fc * 128
                for kc in range(KC):
                    P.add("pe", lambda e, kc=kc, col=col, pb=pb, part=part: e.matmul(
                        pb[:, part, :], lhsT=w1[:, kc, col:col + 128], rhs=hT[:, kc, :], start=(kc == 0), stop=(kc == KC - 1)),
                        reads=[b_w1, b_hT], writes=[bpb], nosync_same=True)
            sgb = sg[fc % 2]; bsg = b_sg[fc % 2]
            P.add("act", lambda e, pb=pb, sgb=sgb: e.activation(out=sgb[:], in_=pb[:, 0, :], func=AF.Silu), reads=[bpb], writes=[bsg])
            P.add("dve", lambda e, pb=pb, sgb=sgb, fc=fc: e.tensor_tensor(out=uT[:, fc, :], in0=sgb[:], in1=pb[:, 1, :], op=ALU.mult),
                  reads=[bpb, bsg], writes=[b_uT])
        for j in range(JT):
            pj = po[j % 2]; bpj = b_po[j % 2]
            for nb in range(2):
                for fc in range(FC):
                    P.add("pe", lambda e, fc=fc, nb=nb, j=j, pj=pj: e.matmul(
                        pj[:, nb * 512:(nb + 1) * 512], lhsT=uT[:, fc, j * 128:(j + 1) * 128], rhs=w2[:, fc, nb * 512:(nb + 1) * 512],
                        start=(fc == 0), stop=(fc == FC - 1)),
                        reads=[b_uT, b_w2], writes=[bpj], nosync_same=True)
            post(pj, bpj, mods[:, 3072:4096], xs[:, j, :], bx[j])
            r0 = (s * JT + j) * 128
            outs.append(P.add("sp", lambda e, j=j, r0=r0, xs=xs: e.dma_start(out=y[r0:r0 + 128, :], in_=xs[:, j, :]), reads=[bx[j]], dma=True))
    P.finish_on("sp", outs)
    P.emit()
    P.close()
    return nc


def f_inputs(x_tok, ym_tok, c_b, mod_w_i, mod_b_i, gpost_mix, gpre_ffn, gpost_ffn, wo, w_in, w_out):
    return dict(
        x=np.ascontiguousarray(x_tok), ym=np.ascontiguousarray(ym_tok),
        cT=np.ascontiguousarray(c_b.reshape(8, 128).T), ident=np.eye(128, dtype=np.float32),
        modw=np.ascontiguousarray(mod_w_i[:, 2048:6144]), modb=np.ascontiguousarray(mod_b_i[None, 2048:6144]),
        gvec=np.ascontiguousarray(np.stack([gpost_mix, gpre_ffn, gpost_ffn])),
        wo=np.ascontiguousarray(wo), w_in=np.ascontiguousarray(w_in), w_out=np.ascontiguousarray(w_out))


def emit_rope(P, src3, dst3, cs_ap, b_src, b_dst, b_cs, tmp4, b_tmp4, nh):
    cosb = cs_ap[:, 0, :].unsqueeze(1).broadcast_to([128, nh, 8])
    sinb = cs_ap[:, 1, :].unsqueeze(1).broadcast_to([128, nh, 8])
    x1 = src3[:, :, 0:8]
    x2 = src3[:, :, 8:16]
    t = [tmp4[:, k, 0:nh * 8].rearrange("p (h d) -> p h d", h=nh) for k in range(4)]
    P.add("act", lambda e: e.copy(out=dst3[:, :, 16:64], in_=src3[:, :, 16:64]), reads=[b_src], writes=[b_dst])
    P.add("dve", lambda e: e.tensor_tensor(out=t[0], in0=x1, in1=cosb, op=ALU.mult), reads=[b_src, b_cs], writes=[b_tmp4])
    P.add("dve", lambda e: e.tensor_tensor(out=t[1], in0=x2, in1=sinb, op=ALU.mult), reads=[b_src, b_cs], writes=[b_tmp4])
    P.add("dve", lambda e: e.tensor_tensor(out=t[2], in0=x2, in1=cosb, op=ALU.mult), reads=[b_src, b_cs], writes=[b_tmp4])
    P.add("dve", lambda e: e.tensor_tensor(out=t[3], in0=x1, in1=sinb, op=ALU.mult), reads=[b_src, b_cs], writes=[b_tmp4])
    P.add("dve", lambda e: e.tensor_tensor(out=dst3[:, :, 0:8], in0=t[0], in1=t[1], op=ALU.subtract), reads=[b_tmp4], writes=[b_dst])
    P.add("dve", lambda e: e.tensor_tensor(out=dst3[:, :, 8:16], in0=t[2], in1=t[3], op=ALU.add), reads=[b_tmp4], writes=[b_dst])


NCMP = 511


def build_kv(S):
    nc = bass.Bass("TRN2", target_bir_lowering=False)
    dram = lambda n, s, dt=F32, kind="ExternalInput": nc.dram_tensor(n, list(s), dt, kind=kind).ap()
    x = dram("x", [S, D])
    cT = dram("cT", [128, 8])
    ident = dram("ident", [128, 128])
    modw = dram("modw", [D, 2048])
    modb = dram("modb", [1, 2048])
    gpre = dram("gpre", [1, D])
    wkv = dram("wkv", [D, 768])
    ropet = dram("ropet", [S, 2, 8])
    w1d = [dram(f"w1_{n}", [64, 32, 256]) for n in "kv"]
    w2d = [dram(f"w2_{n}", [128, 2, 64]) for n in "kv"]
    posd = [dram(f"posT_{n}", [64, 32]) for n in "kv"]
    ncmp = (S - 32) // 16 + 1
    KsT = dram("KsT", [128, S], BF16, kind="ExternalOutput")
    KwT = dram("KwT", [128, S], BF16, kind="ExternalOutput")
    Vs = dram("Vs", [S, 128], BF16, kind="ExternalOutput")
    Vw = dram("Vw", [S, 128], BF16, kind="ExternalOutput")
    KcT = dram("KcT", [128, 512], BF16, kind="ExternalOutput")
    Vc = dram("Vc", [512, 128], BF16, kind="ExternalOutput")

    P = Prog(nc)
    cx = Ctx(P, ident)
    tp = P.ps("tp", [128, 8, 128], BF16); b_tp = P.buf()
    pA = P.ps("pA", [128, 512], F32); b_pA = P.buf()
    pB = P.ps("pB", [128, 512], F32); b_pB = P.buf()
    pT2 = P.ps("pT2", [128, 4, 128], BF16); b_pT2 = P.buf()
    ph = [P.ps(f"ph{i}", [128, 512], F32) for i in range(2)]; b_ph = P.bufs(2)
    pk = P.ps("pk", [128, 512], F32); b_pk = P.buf()

    w = P.sb("w", [128, 8, 768], BF16); b_w = P.buf()
    w_v = wkv.rearrange("(kc p) n -> p kc n", p=128)
    P.add("pool", lambda e: e.dma_start(out=w[:], in_=w_v), writes=[b_w], dma=True)
    mods, b_mods = emit_mods(P, cx, cT, modw, modb, 2048, pA[:, 0:512], b_pA, "kv")
    gp = P.sb("gp", [128, D], F32); b_gp = P.buf()
    P.add("sp", lambda e: e.dma_start(out=gp[:], in_=gpre.broadcast_to([128, D])), writes=[b_gp], dma=True)
    P.add("dve", lambda e: e.scalar_tensor_tensor(out=gp[:], in0=mods[:, 1024:2048], scalar=1.0, in1=gp[:], op0=ALU.add, op1=ALU.mult),
          reads=[b_mods, b_gp], writes=[b_gp])
    ada = AdaLN(P, cx, gp[:], mods[:, 0:1024], b_gp, tp, b_tp, "kv")
    xt = [P.sb(f"xt{i}", [128, D], F32) for i in range(2)]; b_xt = P.bufs(2)
    hT = P.sb("hT", [128, 8, 128], BF16); b_hT = P.buf()
    kcT2 = P.sb("kcT2", [128, S], BF16); vcT2 = P.sb("vcT2", [128, S], BF16); b_cT = P.buf()
    cvb = P.sb("cvb", [128, 256], BF16); b_cvb = P.buf()
    kf = [P.sb(f"kf{i}", [128, 128], F32) for i in range(2)]; b_kf = P.bufs(2)
    kr = [P.sb(f"kr{i}", [128, 128], BF16) for i in range(2)]; b_kr = P.bufs(2)
    kT = [P.sb(f"kT{i}", [128, 2, 128], BF16) for i in range(2)]; b_kT = P.bufs(2)
    vb = [P.sb(f"vb{i}", [128, 2, 128], BF16) for i in range(2)]; b_vb = P.bufs(2)
    cst = [P.sb(f"cst{i}", [128, 2, 8], F32) for i in range(2)]; b_cst = P.bufs(2)
    tmp4 = P.sb("tmp4", [128, 4, 64], F32); b_tmp4 = P.buf()
    outs = []
    for ti in range(S // 128):
        r0 = ti * 128
        xj = xt[ti % 2]; bxj = b_xt[ti % 2]
        cs_t = cst[ti % 2]; bcs = b_cst[ti % 2]
        P.add("sp", lambda e, xj=xj, r0=r0: e.dma_start(out=xj[:], in_=x[r0:r0 + 128, :]), writes=[bxj], dma=True)
        P.add("sp", lambda e, cs_t=cs_t, r0=r0: e.dma_start(out=cs_t[:], in_=ropet[r0:r0 + 128, :, :]), writes=[bcs], dma=True)
        ada.emit(xj[:], bxj, hT[:], b_hT)
        for kc in range(8):
            P.add("pe", lambda e, kc=kc: e.matmul(pA[:, :], lhsT=hT[:, kc, :], rhs=w[:, kc, 0:512], start=(kc == 0), stop=(kc == 7)),
                  reads=[b_w, b_hT], writes=[b_pA], nosync_same=True)
        for kc in range(8):
            P.add("pe", lambda e, kc=kc: e.matmul(pB[:, 0:256], lhsT=hT[:, kc, :], rhs=w[:, kc, 512:768], start=(kc == 0), stop=(kc == 7)),
                  reads=[b_w, b_hT], writes=[b_pB], nosync_same=True)
        P.add("act", lambda e: e.copy(out=cvb[:], in_=pA[:, 0:256]), reads=[b_pA], writes=[b_cvb])
        for i in range(2):
            P.add("pe", lambda e, i=i: e.transpose(out=pT2[:, i, :], in_=cvb[:, i * 128:(i + 1) * 128], identity=cx.idb[:]),
                  reads=[b_cvb, cx.b_idb], writes=[b_pT2], nosync_same=True)
        P.add("act", lambda e, r0=r0: e.copy(out=kcT2[:, r0:r0 + 128], in_=pT2[:, 0, :]), reads=[b_pT2], writes=[b_cT])
        P.add("act", lambda e, r0=r0: e.copy(out=vcT2[:, r0:r0 + 128], in_=pT2[:, 1, :]), reads=[b_pT2], writes=[b_cT])
        kTt = kT[ti % 2]; bkT = b_kT[ti % 2]
        vbt = vb[ti % 2]; bvb = b_vb[ti % 2]
        for i, (src, bsrc) in enumerate([(pA[:, 256:384], b_pA), (pB[:, 0:128], b_pB)]):
            P.add("act", lambda e, i=i, src=src: e.copy(out=kf[i][:], in_=src), reads=[bsrc], writes=[b_kf[i]])
            emit_rope(P, kf[i][:].rearrange("p (h d) -> p h d", h=2), kr[i][:].rearrange("p (h d) -> p h d", h=2), cs_t[:], b_kf[i], b_kr[i], bcs, tmp4, b_tmp4, 2)
            P.add("pe", lambda e, i=i: e.transpose(out=pT2[:, 2 + i, :], in_=kr[i][:], identity=cx.idb[:]), reads=[b_kr[i], cx.b_idb], writes=[b_pT2])
            P.add("act", lambda e, i=i, kTt=kTt: e.copy(out=kTt[:, i, :], in_=pT2[:, 2 + i, :]), reads=[b_pT2], writes=[bkT])
        P.add("act", lambda e, vbt=vbt: e.copy(out=vbt[:, 0, :], in_=pA[:, 384:512]), reads=[b_pA], writes=[bvb])
        P.add("act", lambda e, vbt=vbt: e.copy(out=vbt[:, 1, :], in_=pB[:, 128:256]), reads=[b_pB], writes=[bvb])
        outs.append(P.add("sp", lambda e, kTt=kTt, r0=r0: e.dma_start(out=KsT[:, r0:r0 + 128], in_=kTt[:, 0, :]), reads=[bkT], dma=True))
        outs.append(P.add("sp", lambda e, kTt=kTt, r0=r0: e.dma_start(out=KwT[:, r0:r0 + 128], in_=kTt[:, 1, :]), reads=[bkT], dma=True))
        outs.append(P.add("sp", lambda e, vbt=vbt, r0=r0: e.dma_start(out=Vs[r0:r0 + 128, :], in_=vbt[:, 0, :]), reads=[bvb], dma=True))
        outs.append(P.add("sp", lambda e, vbt=vbt, r0=r0: e.dma_start(out=Vw[r0:r0 + 128, :], in_=vbt[:, 1, :]), reads=[bvb], dma=True))
    w1 = P.sb("w1", [128, 32, 256], BF16); b_w1 = P.buf()
    w2 = P.sb("w2", [128, 2, 64], BF16); b_w2 = P.buf()
    pos = P.sb("pos", [128, 32], F32); posb = P.sb("posb", [128, 32], BF16); b_pos = P.buf()
    biasc = P.sb("biasc", [128, 2], F32); b_bias = P.buf()
    hidT = P.sb("hidT", [128, 2, 512], BF16); b_hid = P.bufs(2)
    kco = P.sb("kco", [128, 512], BF16); b_kco = P.buf()
    vco = P.sb("vco", [128, 4, 128], BF16); b_vco = P.buf()
    kct = P.sb("kct", [128, 4, 128], BF16); b_kct = P.buf()
    P.add("pool", lambda e: e.memset(kct[:], 0.0), writes=[b_kct])
    P.add("pool", lambda e: e.memset(vco[:], 0.0), writes=[b_vco])
    P.add("pool", lambda e: e.memset(hidT[:], 0.0), writes=b_hid)
    for tn in range(2):
        srcT = (kcT2, vcT2)[tn]
        for half in range(2):
            P.add("pool", lambda e, half=half, tn=tn: e.dma_start(out=w1[64 * half:64 * half + 64, :, :], in_=w1d[tn]), writes=[b_w1], dma=True)
            P.add("sp", lambda e, half=half, tn=tn: e.dma_start(out=pos[64 * half:64 * half + 64, :], in_=posd[tn]), writes=[b_pos], dma=True)
        P.add("pool", lambda e, tn=tn: e.dma_start(out=w2[:], in_=w2d[tn]), writes=[b_w2], dma=True)
        P.add("dve", lambda e: e.tensor_copy(out=posb[:], in_=pos[:]), reads=[b_pos], writes=[b_pos])
        for hc in range(2):
            for l in range(32):
                P.add("pe", lambda e, hc=hc, l=l: e.matmul(pk[:, hc:hc + 1], lhsT=w1[0:64, l, hc * 128:(hc + 1) * 128], rhs=posb[0:64, l:l + 1], start=(l == 0), stop=(l == 31)),
                      reads=[b_w1, b_pos], writes=[b_pk], nosync_same=True)
        P.add("dve", lambda e: e.tensor_copy(out=biasc[:], in_=pk[:, 0:2]), reads=[b_pk], writes=[b_bias])
        src3 = srcT[:].rearrange("p (c u) -> p c u", u=16)
        for kvh in range(2):
            ps_ = slice(64 * kvh, 64 * kvh + 64)
            for hc in range(2):
                phb = ph[hc]; bph = b_ph[hc]
                for l in range(32):
                    P.add("pe", lambda e, hc=hc, l=l, ps_=ps_, phb=phb, src3=src3: e.matmul(phb[:, 0:ncmp], lhsT=w1[ps_, l, hc * 128:(hc + 1) * 128],
                                                                         rhs=src3[ps_, (l // 16):(l // 16) + ncmp, l % 16], start=(l == 0), stop=(l == 31)),
                          reads=[b_w1, b_cT], writes=[bph], nosync_same=True)
                P.add("act", lambda e, hc=hc, phb=phb: e.activation(out=hidT[:, hc, 0:ncmp], in_=phb[:, 0:ncmp], func=AF.Silu, bias=biasc[:, hc:hc + 1]),
                      reads=[bph, b_bias], writes=[b_hid[hc]])
            dst = (kct, vco)[tn]
            bdst = (b_kct, b_vco)[tn]
            for cc in range((ncmp + 127) // 128):
                n = min(128, ncmp - cc * 128)
                for hc in range(2):
                    P.add("pe", lambda e, hc=hc, cc=cc, n=n: e.matmul(pk[0:n, 0:64], lhsT=hidT[:, hc, cc * 128:cc * 128 + n], rhs=w2[:, hc, :], start=(hc == 0), stop=(hc == 1)),
                          reads=[b_w2, b_hid[hc]], writes=[b_pk], nosync_same=True)
                P.add("act", lambda e, cc=cc, n=n, kvh=kvh, dst=dst: e.copy(out=dst[0:n, cc, 64 * kvh:64 * kvh + 64], in_=pk[0:n, 0:64]), reads=[b_pk], writes=[bdst])
        if tn == 0:
            for cc in range(4):
                P.add("pe", lambda e, cc=cc: e.transpose(out=pT2[:, cc, :], in_=kct[:, cc, :], identity=cx.idb[:]), reads=[b_kct, cx.b_idb], writes=[b_pT2], nosync_same=True)
            P.add("act", lambda e: e.copy(out=kco[:], in_=pT2[:].rearrange("p a b -> p (a b)")), reads=[b_pT2], writes=[b_kco])
    outs.append(P.add("sp", lambda e: e.dma_start(out=KcT, in_=kco[:]), reads=[b_kco], dma=True))
    outs.append(P.add("sp", lambda e: e.dma_start(out=Vc.rearrange("(cc p) n -> p cc n", p=128), in_=vco[:]), reads=[b_vco], dma=True))
    P.finish_on("sp", outs)
    P.emit()
    P.close()
    return nc


def rope_table(S):
    half = 8
    inv_freq = 500000.0 ** (-np.arange(half, dtype=np.float32) / half)
    ang = np.arange(S, dtype=np.float32)[:, None] * inv_freq[None, :]
    return np.ascontiguousarray(np.stack([np.cos(ang), np.sin(ang)], axis=1).astype(np.float32))


def kv_inputs(x_b, c_b, kv_mod_w, kv_mod_b, kv_norm, w_kv, cmp_pos_k, cmp_w1_k, cmp_w2_k, cmp_pos_v, cmp_w1_v, cmp_w2_v, hp):
    S = x_b.shape[0]
    cols = np.concatenate([i * 256 + (2 * hp + kvh) * 64 + np.arange(64) for i in range(6) for kvh in range(2)])
    d = dict(
        x=np.ascontiguousarray(x_b), cT=np.ascontiguousarray(c_b.reshape(8, 128).T), ident=np.eye(128, dtype=np.float32),
        modw=np.ascontiguousarray(kv_mod_w), modb=np.ascontiguousarray(kv_mod_b[None, :]), gpre=np.ascontiguousarray(kv_norm[None, :]),
        wkv=np.ascontiguousarray(w_kv[:, cols]), ropet=rope_table(S),
        w1_k=np.ascontiguousarray(cmp_w1_k.reshape(32, 64, 256).transpose(1, 0, 2)), w1_v=np.ascontiguousarray(cmp_w1_v.reshape(32, 64, 256).transpose(1, 0, 2)),
        w2_k=np.ascontiguousarray(cmp_w2_k.reshape(2, 128, 64).transpose(1, 0, 2)), w2_v=np.ascontiguousarray(cmp_w2_v.reshape(2, 128, 64).transpose(1, 0, 2)),
        posT_k=np.ascontiguousarray(cmp_pos_k.T), posT_v=np.ascontiguousarray(cmp_pos_v.T))
    return d


SCALE = 0.125


def build_attn(S):
    NT = S // 128
    nc = bass.Bass("TRN2", target_bir_lowering=False)
    dram = lambda n, s, dt=F32, kind="ExternalInput": nc.dram_tensor(n, list(s), dt, kind=kind).ap()
    x = dram("x", [S, D])
    cT = dram("cT", [128, 8])
    ident = dram("ident", [128, 128])
    modw = dram("modw", [D, 2048])
    modb = dram("modb", [1, 2048])
    gpre = dram("gpre", [1, D])
    wq_d = dram("wq", [D, 536])
    ropet = dram("ropet", [S, 2, 8])
    KsT = dram("KsT", [128, S], BF16)
    KwT = dram("KwT", [128, S], BF16)
    Vs = dram("Vs", [S, 128], BF16)
    Vw = dram("Vw", [S, 128], BF16)
    KcT = dram("KcT", [128, 512], BF16)
    Vc = dram("Vc", [512, 128], BF16)
    tri_d = dram("tri", [128, 128])
    tailm_d = dram("tailm", [128, 8])
    c12_d = dram("c12", [NT, 128, 2, 128])
    ex_d = dram("ex", [128, NT, 128])
    o = dram("o", [S, 512], BF16, kind="ExternalOutput")

    P = Prog(nc)
    cx = Ctx(P, ident)
    tp = P.ps("tp", [128, 8, 128], BF16); b_tp = P.buf()
    pA = P.ps("pA", [128, 512], F32); b_pA = P.buf()
    pmx = [P.ps(f"pmx{i}", [128, 4, 128], F32) for i in range(2)]; b_pmx = P.bufs(2)
    pS = [P.ps(f"pS{i}", [128, 512], F32) for i in range(2)]; b_pS = P.bufs(2)
    pacc = P.ps("pacc", [128, 512], F32); b_pacc = P.buf()
    pfin = P.ps("pfin", [128, 4, 128], F32); b_pfin = P.buf()

    KsT2 = P.sb("KsT2", [64, 2, S], BF16); KwT2 = P.sb("KwT2", [64, 2, S], BF16); b_K = P.buf()
    KcT2 = P.sb("KcT2", [64, 2, 512], BF16)
    for kvh in range(2):
        P.add("sp", lambda e, kvh=kvh: e.dma_start(out=KsT2[:, kvh, :], in_=KsT[64 * kvh:64 * kvh + 64, :]), writes=[b_K], dma=True)
        P.add("sp", lambda e, kvh=kvh: e.dma_start(out=KwT2[:, kvh, :], in_=KwT[64 * kvh:64 * kvh + 64, :]), writes=[b_K], dma=True)
        P.add("sp", lambda e, kvh=kvh: e.dma_start(out=KcT2[:, kvh, :], in_=KcT[64 * kvh:64 * kvh + 64, :]), writes=[b_K], dma=True)
    Vs_sb = P.sb("Vs_sb", [128, NT, 2, 65], BF16); Vw_sb = P.sb("Vw_sb", [128, NT, 2, 65], BF16); b_V = P.buf()
    Vc_sb = P.sb("Vc_sb", [128, 4, 2, 64], BF16)
    P.add("pool", lambda e: e.memset(Vs_sb[:], 1.0), writes=[b_V])
    P.add("pool", lambda e: e.memset(Vw_sb[:], 1.0), writes=[b_V])
    Vs_v = Vs.rearrange("(kt p) (h d) -> p kt h d", p=128, h=2)
    Vw_v = Vw.rearrange("(kt p) (h d) -> p kt h d", p=128, h=2)
    for k0 in range(0, NT, 8):
        k1 = min(NT, k0 + 8)
        for kvh in range(2):
            P.add("sp", lambda e, k0=k0, k1=k1, kvh=kvh: e.dma_start(out=Vs_sb[:, k0:k1, kvh, 0:64], in_=Vs_v[:, k0:k1, kvh, :]), writes=[b_V], dma=True)
            P.add("sp", lambda e, k0=k0, k1=k1, kvh=kvh: e.dma_start(out=Vw_sb[:, k0:k1, kvh, 0:64], in_=Vw_v[:, k0:k1, kvh, :]), writes=[b_V], dma=True)
    P.add("sp", lambda e: e.dma_start(out=Vc_sb[:], in_=Vc.rearrange("(cc p) (h d) -> p cc h d", p=128, h=2)), writes=[b_V], dma=True)
    ex_sb = P.sb("ex_sb", [128, NT, 128], BF16); b_ex = P.buf()
    for k0 in range(0, NT, 16):
        k1 = min(NT, k0 + 16)
        P.add("pool", lambda e, k0=k0, k1=k1: e.dma_start(out=ex_sb[:, k0:k1, :], in_=ex_d[:, k0:k1, :]), writes=[b_ex], dma=True)
    trif = P.sb("trif", [128, 128], F32); tri_b = P.sb("tri_b", [128, 128], BF16); tris_b = P.sb("tris_b", [128, 128], BF16); b_tri = P.buf()
    tailm = P.sb("tailm", [128, 8], F32)
    P.add("sp", lambda e: e.dma_start(out=trif[:], in_=tri_d), writes=[b_tri], dma=True)
    P.add("sp", lambda e: e.dma_start(out=tailm[:], in_=tailm_d), writes=[b_tri], dma=True)
    P.add("dve", lambda e: e.tensor_copy(out=tri_b[:], in_=trif[:]), reads=[b_tri], writes=[b_tri])
    P.add("dve", lambda e: e.tensor_scalar(out=tris_b[:], in0=trif[:], scalar1=-1.0, scalar2=1.0, op0=ALU.mult, op1=ALU.add), reads=[b_tri], writes=[b_tri])
    wq = P.sb("wq", [128, 8, 536], BF16); b_wq = P.buf()
    P.add("pool", lambda e: e.dma_start(out=wq[:], in_=wq_d.rearrange("(kc p) n -> p kc n", p=128)), writes=[b_wq], dma=True)
    mods, b_mods = emit_mods(P, cx, cT, modw, modb, 2048, pA[:, 0:512], b_pA, "a")
    gp = P.sb("gp", [128, D], F32); b_gp = P.buf()
    P.add("sp", lambda e: e.dma_start(out=gp[:], in_=gpre.broadcast_to([128, D])), writes=[b_gp], dma=True)
    P.add("dve", lambda e: e.scalar_tensor_tensor(out=gp[:], in0=mods[:, 1024:2048], scalar=1.0, in1=gp[:], op0=ALU.add, op1=ALU.mult),
          reads=[b_mods, b_gp], writes=[b_gp])
    ada = AdaLN(P, cx, gp[:], mods[:, 0:1024], b_gp, tp, b_tp, "a")

    xt = [P.sb(f"xt{i}", [128, D], F32) for i in range(2)]; b_xt = P.bufs(2)
    cst = [P.sb(f"cst{i}", [128, 2, 8], F32) for i in range(2)]; b_cst = P.bufs(2)
    c12 = [P.sb(f"c12_{i}", [128, 2, 128], F32) for i in range(2)]; b_c12 = P.bufs(2)
    hT = P.sb("hT", [128, 8, 128], BF16); b_hT = P.buf()
    qf = P.sb("qf", [128, 512], F32); b_qf = P.buf()
    qr_b = P.sb("qr_b", [128, 512], BF16); b_qr = P.buf()
    qc_b = P.sb("qc_b", [128, 512], BF16); b_qc = P.buf()
    QT = P.sb("QT", [64, 16, 128], BF16); b_QT = P.buf()
    gts = P.sb("gts", [128, 24], F32); b_gts = P.buf()
    tmp4 = P.sb("tmp4", [128, 4, 64], F32); b_tmp4 = P.buf()
    ee = [P.sb(f"ee{i}", [128, 512], F32) for i in range(2)]; b_ee = P.bufs(2)
    sm = [P.sb(f"sm{i}", [128, 4], F32) for i in range(2)]; b_sm = P.bufs(2)
    pgs = P.sb("pgs", [128, 512], F32); b_pgs = P.buf()
    pb = [P.sb(f"pb{i}", [128, 512], BF16) for i in range(2)]; b_pb = P.bufs(2)
    pTc = [P.sb(f"pTc{i}", [128, 4, 128], BF16) for i in range(2)]; b_pTc = P.bufs(2)
    s4 = P.sb("s4", [128, 128], F32); tt = P.sb("tt", [128, 128], F32); imp = P.sb("imp", [128, 128], F32); sc2 = P.sb("sc2", [128, 128], F32)
    b_imp = P.buf()
    m8 = P.sb("m8", [128, 16], F32); b_m8 = P.buf()
    Mblk = P.sb("Mblk", [128, 128], BF16); b_Mblk = P.buf()
    MT = P.sb("MT", [128, 128], BF16); b_MT = P.buf()
    NPT = 3
    pTs = [P.sb(f"pTs{i}", [128, 512], BF16) for i in range(NPT)]; b_pTs = P.bufs(NPT)
    oTs = P.sb("oTs", [128, 512], F32); b_oTs = P.buf()
    fin = P.sb("fin", [128, 8], F32); b_fin = P.buf()
    o_acc = P.sb("o_acc", [128, 512], F32); b_oacc = P.buf()
    ob = [P.sb(f"ob{i}", [128, 512], BF16) for i in range(2)]; b_ob = P.bufs(2)
    P.add("pool", lambda e: e.memset(pgs[:], 0.0), writes=[b_pgs])

    outs = []
    pti = [0]
    mxi = [0]

    def branch(qt, kvh, KT2, V_sb, kts, maskfn, br):
        n = len(kts)

        def scores(idx):
            kt = kts[idx]
            ps = pS[idx % 2]; bps = b_pS[idx % 2]
            P.add("pe", lambda e, kt=kt, ps=ps: e.matmul(ps[:, :], lhsT=KT2[:, kvh, kt * 128:(kt + 1) * 128],
                                                  rhs=QT[:, kvh * 4:kvh * 4 + 4, :].rearrange("p a b -> p (a b)"), start=True, stop=True),
                  reads=[b_K, b_QT], writes=[bps])
        scores(0)
        for idx in range(n):
            kt = kts[idx]
            if idx + 1 < n:
                scores(idx + 1)
            ps = pS[idx % 2]; bps = b_pS[idx % 2]
            pt = pTs[pti[0] % NPT]; bpt = b_pTs[pti[0] % NPT]; pti[0] += 1
            P.add("act", lambda e, ps=ps, pt=pt: e.activation(out=pt[:], in_=ps[:, :], func=AF.Exp, scale=SCALE), reads=[bps], writes=[bpt])
            m = maskfn(idx, kt)
            if m is not None:
                m_ap, m_bufs = m
                P.add("dve", lambda e, pt=pt, m_ap=m_ap: e.tensor_tensor(out=pt[:].rearrange("p (a q) -> p a q", a=4), in0=pt[:].rearrange("p (a q) -> p a q", a=4),
                                                                 in1=m_ap.unsqueeze(1).broadcast_to([128, 4, 128]), op=ALU.mult),
                      reads=[bpt] + m_bufs, writes=[bpt])
            P.add("pe", lambda e, kt=kt, pt=pt, idx=idx: e.matmul(pacc[0:65, :], lhsT=V_sb[:, kt, kvh, :], rhs=pt[:], start=(idx == 0), stop=(idx == n - 1)),
                  reads=[b_V, bpt], writes=[b_pacc], nosync_same=(idx > 0))
        P.add("act", lambda e: e.copy(out=oTs[0:65, :], in_=pacc[0:65, :]), reads=[b_pacc], writes=[b_oTs])
        for hs in range(4):
            P.add("pe", lambda e, hs=hs: e.transpose(out=pfin[:, hs, 0:65], in_=oTs[0:65, hs * 128:(hs + 1) * 128], identity=cx.idf[0:65, 0:65]),
                  reads=[b_oTs, cx.b_idf], writes=[b_pfin], nosync_same=(hs > 0))
        P.add("dve", lambda e: e.reciprocal(out=fin[:, 0:4], in_=pfin[:, :, 64]), reads=[b_pfin], writes=[b_fin])
        for hs in range(4):
            hcol = (kvh * 4 + hs)
            P.add("dve", lambda e, hs=hs, hcol=hcol: e.tensor_tensor(out=fin[:, 4 + hs:5 + hs], in0=fin[:, hs:hs + 1], in1=gts[:, hcol * 3 + br:hcol * 3 + br + 1], op=ALU.mult),
                  reads=[b_fin, b_gts], writes=[b_fin])
            P.add("dve", lambda e, hs=hs, hcol=hcol: e.scalar_tensor_tensor(out=o_acc[:, hcol * 64:(hcol + 1) * 64], in0=pfin[:, hs, 0:64], scalar=fin[:, 4 + hs:5 + hs],
                                                                    in1=o_acc[:, hcol * 64:(hcol + 1) * 64], op0=ALU.mult, op1=ALU.add),
                  reads=[b_pfin, b_fin, b_oacc], writes=[b_oacc])

    for qt in range(NT):
        r0 = qt * 128
        xj = xt[qt % 2]; bxj = b_xt[qt % 2]
        cs_t = cst[qt % 2]; bcs = b_cst[qt % 2]
        c12t = c12[qt % 2]; bc12 = b_c12[qt % 2]
        P.add("sp", lambda e, xj=xj, r0=r0: e.dma_start(out=xj[:], in_=x[r0:r0 + 128, :]), writes=[bxj], dma=True)
        P.add("sp", lambda e, cs_t=cs_t, r0=r0: e.dma_start(out=cs_t[:], in_=ropet[r0:r0 + 128, :, :]), writes=[bcs], dma=True)
        P.add("sp", lambda e, c12t=c12t, qt=qt: e.dma_start(out=c12t[:], in_=c12_d[qt]), writes=[bc12], dma=True)
        ada.emit(xj[:], bxj, hT[:], b_hT)
        for kc in range(8):
            P.add("pe", lambda e, kc=kc: e.matmul(pA[:, :], lhsT=hT[:, kc, :], rhs=wq[:, kc, 0:512], start=(kc == 0), stop=(kc == 7)),
                  reads=[b_wq, b_hT], writes=[b_pA], nosync_same=True)
        for kc in range(8):
            P.add("pe", lambda e, kc=kc: e.matmul(pfin[:, 0, 0:24], lhsT=hT[:, kc, :], rhs=wq[:, kc, 512:536], start=(kc == 0), stop=(kc == 7)),
                  reads=[b_wq, b_hT], writes=[b_pfin], nosync_same=True)
        P.add("act", lambda e: e.activation(out=gts[:], in_=pfin[:, 0, 0:24], func=AF.Sigmoid), reads=[b_pfin], writes=[b_gts])
        P.add("act", lambda e: e.copy(out=qf[:], in_=pA[:, :]), reads=[b_pA], writes=[b_qf])
        P.add("pool", lambda e: e.tensor_copy(out=qc_b[:], in_=qf[:]), reads=[b_qf], writes=[b_qc])
        emit_rope(P, qf[:].rearrange("p (h d) -> p h d", h=8), qr_b[:].rearrange("p (h d) -> p h d", h=8), cs_t[:], b_qf, b_qr, bcs, tmp4, b_tmp4, 8)
        for rr, (qsrc, bq) in enumerate([(qr_b, b_qr), (qc_b, b_qc)]):
            for h_ in range(8):
                P.add("pe", lambda e, h_=h_, qsrc=qsrc: e.transpose(out=tp[0:64, h_, :], in_=qsrc[:, h_ * 64:(h_ + 1) * 64], identity=cx.idb[:]),
                      reads=[bq, cx.b_idb], writes=[b_tp], nosync_same=(h_ > 0))
            P.add("act", lambda e, rr=rr: e.copy(out=QT[:, rr * 8:(rr + 1) * 8, :], in_=tp[0:64, :, :]), reads=[b_tp], writes=[b_QT])
        ncv = min(512, 8 * (qt + 1))
        nch = (ncv + 127) // 128
        for kvh in range(2):
            for g in range(4):
                i_, e_ = g // 2, g % 2
                hcol = kvh * 4 + g
                eb = ee[g % 2]; beb = b_ee[g % 2]
                smb = sm[g % 2]; bsm = b_sm[g % 2]
                pbb = pb[g % 2]; bpbb = b_pb[g % 2]
                pTb = pTc[g % 2]; bpTb = b_pTc[g % 2]
                P.add("pe", lambda e, g=g, kvh=kvh, ncv=ncv: e.matmul(pA[:, 0:ncv], lhsT=QT[:, 8 + kvh * 4 + g, :], rhs=KcT2[:, kvh, 0:ncv], start=True, stop=True),
                      reads=[b_QT, b_K], writes=[b_pA])
                P.add("dve", lambda e, smb=smb, ncv=ncv: e.reduce_max(out=smb[:, 0:1], in_=pA[:, 0:ncv], axis=AX.X), reads=[b_pA], writes=[bsm])
                P.add("dve", lambda e, smb=smb: e.tensor_scalar(out=smb[:, 1:2], in0=smb[:, 0:1], scalar1=-SCALE, scalar2=None, op0=ALU.mult), reads=[bsm], writes=[bsm])
                P.add("act", lambda e, eb=eb, smb=smb, ncv=ncv: e.activation(out=eb[:, 0:ncv], in_=pA[:, 0:ncv], func=AF.Exp, scale=SCALE, bias=smb[:, 1:2]),
                      reads=[b_pA, bsm], writes=[beb])
                P.add("dve", lambda e, eb=eb, ncv=ncv: e.tensor_tensor(out=eb[:, ncv - 8:ncv], in0=eb[:, ncv - 8:ncv], in1=tailm[:], op=ALU.mult), reads=[beb, b_tri], writes=[beb])
                P.add("dve", lambda e, eb=eb, smb=smb, ncv=ncv: e.reduce_sum(out=smb[:, 2:3], in_=eb[:, 0:ncv], axis=AX.X), reads=[beb], writes=[bsm])
                P.add("dve", lambda e, smb=smb: e.tensor_scalar(out=smb[:, 2:3], in0=smb[:, 2:3], scalar1=1e-30, scalar2=None, op0=ALU.max), reads=[bsm], writes=[bsm])
                P.add("dve", lambda e, smb=smb: e.reciprocal(out=smb[:, 3:4], in_=smb[:, 2:3]), reads=[bsm], writes=[bsm])
                P.add("act", lambda e, eb=eb, smb=smb, pbb=pbb, ncv=ncv: e.activation(out=pbb[:, 0:ncv], in_=eb[:, 0:ncv], func=AF.Identity, scale=smb[:, 3:4]),
                      reads=[beb, bsm], writes=[bpbb])
                if g == 0:
                    P.add("dve", lambda e, eb=eb, smb=smb, ncv=ncv: e.tensor_scalar(out=pgs[:, 0:ncv], in0=eb[:, 0:ncv], scalar1=smb[:, 3:4], scalar2=None, op0=ALU.mult),
                          reads=[beb, bsm], writes=[b_pgs])
                else:
                    P.add("dve", lambda e, eb=eb, smb=smb, ncv=ncv: e.scalar_tensor_tensor(out=pgs[:, 0:ncv], in0=eb[:, 0:ncv], scalar=smb[:, 3:4], in1=pgs[:, 0:ncv], op0=ALU.mult, op1=ALU.add),
                          reads=[beb, bsm, b_pgs], writes=[b_pgs])
                for ch in range(nch):
                    wd = min(128, ncv - ch * 128)
                    P.add("pe", lambda e, ch=ch, wd=wd, pbb=pbb: e.transpose(out=tp[0:wd, ch, :], in_=pbb[:, ch * 128:ch * 128 + wd], identity=cx.idb[:]),
                          reads=[bpbb, cx.b_idb], writes=[b_tp], nosync_same=(ch > 0))
                P.add("act", lambda e, pTb=pTb, nch=nch: e.copy(out=pTb[:, 0:nch, :], in_=tp[:, 0:nch, :]), reads=[b_tp], writes=[bpTb])
                for ch in range(nch):
                    wd = min(128, ncv - ch * 128)
                    P.add("pe", lambda e, ch=ch, wd=wd, pTb=pTb, g=g, kvh=kvh: e.matmul(pfin[:, g, 0:64], lhsT=pTb[0:wd, ch, :], rhs=Vc_sb[0:wd, ch, kvh, :], start=(ch == 0), stop=(ch == nch - 1)),
                          reads=[bpTb, b_V], writes=[b_pfin], nosync_same=(ch > 0))
            for g in range(4):
                hcol = kvh * 4 + g
                P.add("dve", lambda e, g=g, hcol=hcol: e.tensor_scalar(out=o_acc[:, hcol * 64:(hcol + 1) * 64], in0=pfin[:, g, 0:64], scalar1=gts[:, hcol * 3:hcol * 3 + 1], scalar2=None, op0=ALU.mult),
                      reads=[b_pfin, b_gts], writes=[b_oacc])
            p4 = pgs[:].rearrange("p (j u) -> p j u", u=4)
            P.add("dve", lambda e, p4=p4: e.reduce_sum(out=s4[:], in_=p4, axis=AX.X), reads=[b_pgs], writes=[b_imp])
            P.add("dve", lambda e, p4=p4: e.scalar_tensor_tensor(out=tt[:], in0=s4[:], scalar=2.0, in1=p4[:, :, 3], op0=ALU.mult, op1=ALU.subtract), reads=[b_pgs, b_imp], writes=[b_imp])
            P.add("dve", lambda e, p4=p4: e.tensor_tensor(out=imp[:, 1:128], in0=tt[:, 1:128], in1=p4[:, 0:127, 3], op=ALU.add), reads=[b_pgs, b_imp], writes=[b_imp])
            P.add("dve", lambda e: e.tensor_copy(out=imp[:, 0:1], in_=tt[:, 0:1]), reads=[b_imp], writes=[b_imp])
            P.add("dve", lambda e, c12t=c12t: e.tensor_tensor(out=imp[:], in0=imp[:], in1=c12t[:, 0, :], op=ALU.mult), reads=[b_imp, bc12], writes=[b_imp])
            P.add("dve", lambda e, c12t=c12t: e.tensor_tensor(out=imp[:], in0=imp[:], in1=c12t[:, 1, :], op=ALU.add), reads=[b_imp, bc12], writes=[b_imp])
            P.add("dve", lambda e: e.max(out=m8[:, 0:8], in_=imp[:]), reads=[b_imp], writes=[b_m8])
            P.add("dve", lambda e: e.match_replace(out=sc2[:], in_to_replace=m8[:, 0:8], in_values=imp[:], imm_value=-1e9), reads=[b_imp, b_m8], writes=[b_imp])
            P.add("dve", lambda e: e.max(out=m8[:, 8:16], in_=sc2[:]), reads=[b_imp], writes=[b_m8])
            P.add("dve", lambda e: e.tensor_scalar(out=Mblk[:], in0=imp[:], scalar1=m8[:, 15:16], scalar2=None, op0=ALU.is_ge), reads=[b_imp, b_m8], writes=[b_Mblk])
            P.add("pe", lambda e: e.transpose(out=tp[:, 0, :], in_=Mblk[:], identity=cx.idb[:]), reads=[b_Mblk, cx.b_idb], writes=[b_tp])
            P.add("act", lambda e: e.copy(out=MT[:], in_=tp[:, 0, :]), reads=[b_tp], writes=[b_MT])
            grp = {}

            def sel_mask(idx, kt, qt=qt, grp=grp):
                if kt == qt:
                    return tri_b[:], [b_tri]
                k0 = (kt // 4) * 4
                if k0 not in grp:
                    slot = mxi[0] % 2; mxi[0] += 1
                    for k in range(k0, min(k0 + 4, qt)):
                        P.add("pe", lambda e, k=k, k0=k0, slot=slot: e.matmul(pmx[slot][:, k - k0, :], lhsT=ex_sb[:, k, :], rhs=MT[:], start=True, stop=True),
                              reads=[b_ex, b_MT], writes=[b_pmx[slot]], nosync_same=(k > k0))
                    grp[k0] = slot
                slot = grp[k0]
                return pmx[slot][:, kt - k0, :], [b_pmx[slot]]
            branch(qt, kvh, KsT2, Vs_sb, list(range(0, qt + 1)), sel_mask, 1)

            def win_mask(idx, kt, qt=qt):
                if kt == qt:
                    return tri_b[:], [b_tri]
                if kt == qt - 4:
                    return tris_b[:], [b_tri]
                return None
            branch(qt, kvh, KwT2, Vw_sb, list(range(max(0, qt - 4), qt + 1)), win_mask, 2)
        obj = ob[qt % 2]; bob = b_ob[qt % 2]
        P.add("act", lambda e, obj=obj: e.copy(out=obj[:], in_=o_acc[:]), reads=[b_oacc], writes=[bob])
        outs.append(P.add("sp", lambda e, obj=obj, r0=r0: e.dma_start(out=o[r0:r0 + 128, :], in_=obj[:]), reads=[bob], dma=True))
    P.finish_on("sp", outs)
    P.emit()
    P.close()
    return nc


def attn_consts(S):
    NT = S // 128
    nsb = S // 64
    tri = np.triu(np.ones((128, 128), np.float32))
    q = np.arange(128)
    tailm = (16 * np.arange(8)[None, :] + 31 <= q[:, None]).astype(np.float32)
    c12 = np.zeros((NT, 128, 2, 128), np.float32)
    j = np.arange(128)[None, :]
    for qt in range(NT):
        qblk = ((qt * 128 + q) // 64)[:, None]
        forced = (j == 0) | (j == qblk) | (j == qblk - 1)
        causal = j <= qblk
        c1 = (causal & ~forced).astype(np.float32)
        c2 = np.where(forced, 1e4 + j, np.where(causal, 0.0, np.where(j < nsb, -1.0 - j * 1e-3, -10.0 - j))).astype(np.float32)
        c12[qt, :, 0, :] = c1
        c12[qt, :, 1, :] = c2
    ex = np.zeros((128, NT, 128), np.float32)
    for kt in range(NT):
        ex[2 * kt, kt, 0:64] = 1.0
        ex[2 * kt + 1, kt, 64:128] = 1.0
    return dict(tri=tri, tailm=tailm, c12=c12, ex=ex, ident=np.eye(128, dtype=np.float32), ropet=rope_table(S))


def attn_inputs(x_b, c_b, mod_w_i, mod_b_i, gpre_i, w_q_i, kvres, hp, consts):
    qcols = np.concatenate([np.arange(hp * 512, hp * 512 + 512), 1024 + np.arange(hp * 24, hp * 24 + 24)])
    d = dict(
        x=np.ascontiguousarray(x_b), cT=np.ascontiguousarray(c_b.reshape(8, 128).T),
        modw=np.ascontiguousarray(mod_w_i[:, 0:2048]), modb=np.ascontiguousarray(mod_b_i[None, 0:2048]),
        gpre=np.ascontiguousarray(gpre_i[None, :]), wq=np.ascontiguousarray(w_q_i[:, qcols]))
    for k in ("KsT", "KwT", "Vs", "Vw", "KcT", "Vc"):
        d[k] = kvres[k]
    d.update(consts)
    return d


SEQ = 8192
BATCH = 4
_PROGS = {}


def _prog(key, fn):
    if key not in _PROGS:
        _PROGS[key] = fn()
    return _PROGS[key]


def _run(nc, in_maps):
    res = run_bass_kernel_spmd(nc, in_maps, core_ids=list(range(NCORES)))
    return res.results


def kernel(x, c, mod_w, mod_b, norm_pre_mix, norm_post_mix, norm_pre_ffn, norm_post_ffn,
           ffn_w_in, ffn_w_out, ssm_w_in, ssm_conv_w, ssm_conv_b, ssm_dt_bias, ssm_a_log,
           ssm_d, ssm_norm, ssm_w_out, kv_norm, kv_mod_w, kv_mod_b, w_kv,
           cmp_pos_k, cmp_w1_k, cmp_w2_k, cmp_pos_v, cmp_w1_v, cmp_w2_v, nsa_w_q, nsa_w_o):
    A = lambda a: np.asarray(a)
    x = A(x).astype(np.float32, copy=True)
    c = A(c)
    S = x.shape[1]
    TH = S // 2

    def f_phase(i, ym_parts, wo, KM):
        in_maps = []
        for b in range(BATCH):
            for th in range(2):
                tok = slice(th * TH, (th + 1) * TH)
                ym_tok = np.concatenate([ym_parts[b][0][tok], ym_parts[b][1][tok]], axis=1)
                in_maps.append(f_inputs(x[b, tok], ym_tok, c[b], A(mod_w[i]), A(mod_b[i]), A(norm_post_mix[i]), A(norm_pre_ffn[i]), A(norm_post_ffn[i]),
                                        wo, A(ffn_w_in[i]), A(ffn_w_out[i])))
        res = _run(_prog(("f", TH, KM), lambda: build_f(TH, KM)), in_maps)
        for b in range(BATCH):
            for th in range(2):
                x[b, th * TH:(th + 1) * TH] = np.asarray(res[b * 2 + th]["y"])

    for i in range(2):
        in_maps = [mamba_inputs(x[b], c[b], A(mod_w[i]), A(mod_b[i]), A(norm_pre_mix[i]), A(ssm_w_in[i]), A(ssm_conv_w[i]), A(ssm_conv_b[i]),
                                A(ssm_dt_bias[i]), A(ssm_a_log[i]), A(ssm_d[i]), A(ssm_norm[i]), hp) for b in range(BATCH) for hp in range(2)]
        res = _run(_prog(("m", S), lambda: build_mamba(S)), in_maps)
        parts = [[np.asarray(res[b * 2 + hp]["yn"]) for hp in range(2)] for b in range(BATCH)]
        f_phase(i, parts, A(ssm_w_out[i]), 2048)

    in_maps = [kv_inputs(x[b], c[b], A(kv_mod_w), A(kv_mod_b), A(kv_norm), A(w_kv), A(cmp_pos_k), A(cmp_w1_k), A(cmp_w2_k),
                         A(cmp_pos_v), A(cmp_w1_v), A(cmp_w2_v), hp) for b in range(BATCH) for hp in range(2)]
    kvres = _run(_prog(("kv", S), lambda: build_kv(S)), in_maps)
    consts = attn_consts(S)
    for i in range(2, 4):
        in_maps = [attn_inputs(x[b], c[b], A(mod_w[i]), A(mod_b[i]), A(norm_pre_mix[i]), A(nsa_w_q[i - 2]), kvres[b * 2 + hp], hp, consts)
                   for b in range(BATCH) for hp in range(2)]
        res = _run(_prog(("a", S), lambda: build_attn(S)), in_maps)
        parts = [[np.asarray(res[b * 2 + hp]["o"]) for hp in range(2)] for b in range(BATCH)]
        f_phase(i, parts, A(nsa_w_o[i - 2]), 1024)
    return x
```
